# Optimizing a Trainium2 kernel written in Bass

```python
import math
import jax, jax.numpy as jnp
from jax import lax
import numpy as np

D_MODEL = 2048
BATCH = 1
SEQ = 16384
DEPTH = 4

CHUNK = 64
Q_BLOCK = 128
MIX_WIDTH = D_MODEL
DIFF_WIDTH = MIX_WIDTH // 2
RWKV_WIDTH = MIX_WIDTH - DIFF_WIDTH
DIFF_V_DIM = 128
DIFF_QK_DIM = DIFF_V_DIM // 2
DIFF_HEADS = DIFF_WIDTH // DIFF_V_DIM
DIFF_QK_W = DIFF_HEADS * 2 * DIFF_QK_DIM
DIFF_V_W = DIFF_HEADS * DIFF_V_DIM
RWKV_HEAD_DIM = 64
RWKV_HEADS = RWKV_WIDTH // RWKV_HEAD_DIM
DECAY_LORA = 64
AAA_LORA = 64
MV_LORA = 32
GATE_LORA = 160
RWKV_SHIFT_WIDTH = 3 * RWKV_WIDTH + DECAY_LORA + AAA_LORA + GATE_LORA
PROJ_WIDTH = 2 * DIFF_QK_W + DIFF_V_W + RWKV_SHIFT_WIDTH
D_FF = 4 * D_MODEL
RMS_EPS = 1e-6
RWKV_GN_EPS = 64e-5
NEG_INF = -1e30

kernel_name = "hybrid_diffattn_rwkv7_sqrelu_trunk"


def rms_norm(x, g, eps=RMS_EPS):
    xf = x.astype(jnp.float32)
    y = xf * lax.rsqrt(jnp.mean(xf * xf, axis=-1, keepdims=True) + eps)
    return (y * g.astype(jnp.float32)).astype(x.dtype)


def alibi_slopes(n):
    return jnp.asarray([2.0 ** (-8.0 * (i + 1) / n) for i in range(n)], jnp.float32)


def token_shift(p, mu):
    prev = jnp.pad(p, ((0, 0), (1, 0), (0, 0)))[:, :-1]
    return p + (prev - p) * mu


def diff_attention(q, k, v, lam, slopes):
    B, T, H, _, DK = q.shape
    nb = T // Q_BLOCK
    scale = 1.0 / math.sqrt(DK)
    q_blocks = jnp.moveaxis(q.reshape(B, nb, Q_BLOCK, H, 2, DK), 1, 0)
    k_pos = jnp.arange(T)

    def one_block(args):
        q_blk, blk = args
        q_pos = blk * Q_BLOCK + jnp.arange(Q_BLOCK)
        s = jnp.einsum('bqhcd,bkhcd->bhcqk', q_blk, k) * scale
        dist = jnp.abs(q_pos[:, None] - k_pos[None, :]).astype(jnp.float32)
        allowed = (k_pos[None, :] // CHUNK) <= (q_pos[:, None] // CHUNK)
        s = s - slopes[None, :, None, None, None] * dist
        s = jnp.where(allowed, s, NEG_INF)
        p = jax.nn.softmax(s, axis=-1)
        attn = p[:, :, 0] - lam * p[:, :, 1]
        return jnp.einsum('bhqk,bkhd->bqhd', attn, v)

    out = lax.map(one_block, (q_blocks, jnp.arange(nb)))
    return jnp.moveaxis(out, 0, 1).reshape(B, T, H, v.shape[-1])


def rwkv7_scan(r, w, k, v, a, b):
    B, T, H, N = r.shape

    def step(S, inp):
        r_t, w_t, k_t, v_t, a_t, b_t = inp
        sa = jnp.einsum('bhij,bhj->bhi', S, a_t)
        S = S * w_t[:, :, None, :] + sa[..., None] * b_t[:, :, None, :] + v_t[..., None] * k_t[:, :, None, :]
        y = jnp.einsum('bhij,bhj->bhi', S, r_t)
        return S, y

    xs = tuple(jnp.moveaxis(t, 1, 0) for t in (r, w, k, v, a, b))
    S0 = jnp.zeros((B, H, N, N), jnp.float32)
    _, ys = lax.scan(step, S0, xs)
    return jnp.moveaxis(ys, 0, 1)


def setup_inputs(seed: int = 0) -> dict:
    key = jax.random.key(seed)
    ks = jax.random.split(key, 26)
    f = jnp.float32
    L = DEPTH

    def nrm(k, shape, scale):
        return jax.random.normal(k, shape, f) * scale

    return {
        "x": nrm(ks[0], (BATCH, SEQ, D_MODEL), 1.0),
        "norm_mix": 1.0 + nrm(ks[1], (L, D_MODEL), 0.02),
        "norm_mlp": 1.0 + nrm(ks[2], (L, D_MODEL), 0.02),
        "w_in": nrm(ks[3], (L, D_MODEL, PROJ_WIDTH), D_MODEL ** -0.5),
        "w_out": nrm(ks[4], (L, MIX_WIDTH, D_MODEL), MIX_WIDTH ** -0.5),
        "qk_norm_q": 1.0 + nrm(ks[5], (L, 2, DIFF_QK_DIM), 0.02),
        "qk_norm_k": 1.0 + nrm(ks[6], (L, 2, DIFF_QK_DIM), 0.02),
        "diff_lambda_q": nrm(ks[7], (L, 2, DIFF_QK_DIM), 0.1),
        "diff_lambda_k": nrm(ks[8], (L, 2, DIFF_QK_DIM), 0.1),
        "diff_subln": 1.0 + nrm(ks[9], (L, DIFF_V_DIM), 0.02),
        "rwkv_mu": jax.random.uniform(ks[10], (L, RWKV_SHIFT_WIDTH), f, 0.0, 1.0),
        "rwkv_w0": -0.5 + nrm(ks[11], (L, RWKV_WIDTH), 0.5),
        "rwkv_w2": nrm(ks[12], (L, DECAY_LORA, RWKV_WIDTH), 0.5 * DECAY_LORA ** -0.5),
        "rwkv_a0": nrm(ks[13], (L, RWKV_WIDTH), 0.1),
        "rwkv_a2": nrm(ks[14], (L, AAA_LORA, RWKV_WIDTH), 0.5 * AAA_LORA ** -0.5),
        "rwkv_g2": nrm(ks[15], (L, GATE_LORA, RWKV_WIDTH), GATE_LORA ** -0.5),
        "rwkv_k_k": 0.85 + nrm(ks[16], (L, RWKV_WIDTH), 0.05),
        "rwkv_k_a": 1.0 + nrm(ks[17], (L, RWKV_WIDTH), 0.05),
        "rwkv_r_k": nrm(ks[18], (L, RWKV_HEADS, RWKV_HEAD_DIM), 0.1),
        "rwkv_ln_w": 1.0 + nrm(ks[19], (L, RWKV_WIDTH), 0.02),
        "rwkv_ln_b": nrm(ks[20], (L, RWKV_WIDTH), 0.02),
        "rwkv_v0": nrm(ks[21], (L - 1, RWKV_WIDTH), 0.5),
        "rwkv_v1": nrm(ks[22], (L - 1, RWKV_WIDTH, MV_LORA), RWKV_WIDTH ** -0.5),
        "rwkv_v2": nrm(ks[23], (L - 1, MV_LORA, RWKV_WIDTH), 0.5 * MV_LORA ** -0.5),
        "mlp_up": nrm(ks[24], (L, D_MODEL, D_FF), D_MODEL ** -0.5),
        "mlp_down": nrm(ks[25], (L, D_FF, D_MODEL), D_FF ** -0.5),
    }


def reference(x, norm_mix, norm_mlp, w_in, w_out, qk_norm_q, qk_norm_k, diff_lambda_q,
              diff_lambda_k, diff_subln, rwkv_mu, rwkv_w0, rwkv_w2, rwkv_a0, rwkv_a2, rwkv_g2,
              rwkv_k_k, rwkv_k_a, rwkv_r_k, rwkv_ln_w, rwkv_ln_b, rwkv_v0, rwkv_v1, rwkv_v2,
              mlp_up, mlp_down):
    B, T, _ = x.shape
    f32 = jnp.float32
    slopes = alibi_slopes(DIFF_HEADS)
    v_first = None
    for l in range(DEPTH):
        h = rms_norm(x, norm_mix[l])
        proj = jnp.einsum('btd,de->bte', h, w_in[l])
        q_d, k_d, v_d, rw = jnp.split(
            proj, [DIFF_QK_W, 2 * DIFF_QK_W, 2 * DIFF_QK_W + DIFF_V_W], axis=-1)

        q_d = rms_norm(q_d.reshape(B, T, DIFF_HEADS, 2, DIFF_QK_DIM), qk_norm_q[l]).astype(f32)
        k_d = rms_norm(k_d.reshape(B, T, DIFF_HEADS, 2, DIFF_QK_DIM), qk_norm_k[l]).astype(f32)
        v_d = v_d.reshape(B, T, DIFF_HEADS, DIFF_V_DIM).astype(f32)
        lam_init = 0.8 - 0.6 * math.exp(-0.3 * l)
        lq = diff_lambda_q[l].astype(f32)
        lk = diff_lambda_k[l].astype(f32)
        lam = jnp.exp(jnp.sum(lq[0] * lk[0])) - jnp.exp(jnp.sum(lq[1] * lk[1])) + lam_init
        a_out = diff_attention(q_d, k_d, v_d, lam, slopes)
        a_out = rms_norm(a_out, diff_subln[l]) * (1.0 - lam_init)
        a_out = a_out.reshape(B, T, DIFF_WIDTH).astype(x.dtype)

        rw = token_shift(rw, rwkv_mu[l])
        r, kr, vr, lat_w, lat_a, lat_g = jnp.split(
            rw, [RWKV_WIDTH, 2 * RWKV_WIDTH, 3 * RWKV_WIDTH,
                 3 * RWKV_WIDTH + DECAY_LORA, 3 * RWKV_WIDTH + DECAY_LORA + AAA_LORA], axis=-1)
        w_log = -jax.nn.softplus(-(rwkv_w0[l] + jnp.tanh(lat_w) @ rwkv_w2[l])) - 0.5
        decay = jnp.exp(-jnp.exp(w_log.astype(f32)))
        if l == 0:
            v_first = vr
        else:
            v_gate = jax.nn.sigmoid(rwkv_v0[l - 1] + (vr @ rwkv_v1[l - 1]) @ rwkv_v2[l - 1])
            vr = vr + (v_first - vr) * v_gate
        a_rate = jax.nn.sigmoid(rwkv_a0[l] + lat_a @ rwkv_a2[l])
        g = jax.nn.sigmoid(lat_g) @ rwkv_g2[l]
        heads = (B, T, RWKV_HEADS, RWKV_HEAD_DIM)
        kk = (kr * rwkv_k_k[l]).astype(f32).reshape(heads)
        kk = kk / jnp.maximum(jnp.sqrt(jnp.sum(kk * kk, axis=-1, keepdims=True)), 1e-12)
        a_h = a_rate.astype(f32).reshape(heads)
        k_mod = (kr * (1.0 + (a_rate - 1.0) * rwkv_k_a[l])).astype(f32).reshape(heads)
        r_h = r.astype(f32).reshape(heads)
        v_h = vr.astype(f32).reshape(heads)
        y = rwkv7_scan(r_h, decay.reshape(heads), k_mod, v_h, -kk, kk * a_h)
        mean = jnp.mean(y, axis=-1, keepdims=True)
        var = jnp.mean(jnp.square(y - mean), axis=-1, keepdims=True)
        y = ((y - mean) * lax.rsqrt(var + RWKV_GN_EPS)).reshape(B, T, RWKV_WIDTH)
        y = y * rwkv_ln_w[l].astype(f32) + rwkv_ln_b[l].astype(f32)
        bonus = jnp.sum(r_h * k_mod * rwkv_r_k[l].astype(f32), axis=-1, keepdims=True) * v_h
        y = (y + bonus.reshape(B, T, RWKV_WIDTH)).astype(x.dtype) * g

        mixed = jnp.concatenate([a_out, y], axis=-1)
        x = x + jnp.einsum('bte,ed->btd', mixed, w_out[l])

        h2 = rms_norm(x, norm_mlp[l])
        u = jnp.square(jax.nn.relu(jnp.einsum('btd,df->btf', h2, mlp_up[l])))
        x = x + jnp.einsum('btf,fd->btd', u, mlp_down[l])
    return x
```

```python
import math
import numpy as np
import ml_dtypes
import concourse.bass as bass
import concourse.mybir as mybir
from concourse.bass_utils import run_bass_kernel_spmd

F32 = mybir.dt.float32
BF16 = mybir.dt.bfloat16
ALU = mybir.AluOpType
AF = mybir.ActivationFunctionType

NCORES = 8
D = 2048
T = 16384
TC = T // NCORES
DEPTH = 4
DFF = 8192
NDC = D // 128
NFC = DFF // 128
TT = 512
RMS_EPS = 1e-6
GN_EPS = 64e-5


class Buf:
    __slots__ = ("w", "r", "excl")

    def __init__(self, excl=False):
        self.w = None
        self.r = {}
        self.excl = excl


class Ctx:
    def __init__(self, nc):
        self.nc = nc
        self.eng = {"pe": nc.tensor, "dve": nc.vector, "act": nc.scalar, "pool": nc.gpsimd, "sp": nc.sync}
        self.sems = {}
        self.val = {}
        self.waited = {e: {} for e in self.eng}
        for e in self.eng:
            self._mk(e)
        self.n_ins = 0
        self.chbufs = {}

    def _mk(self, name):
        self.sems[name] = self.nc.alloc_semaphore(name="s_" + name)
        self.val[name] = 0

    def _wait(self, e, deps):
        w = self.waited[e]
        for s, v in deps.items():
            if w.get(s, 0) < v:
                self.eng[e].wait_ge(self.sems[s], v)
                w[s] = v

    def _deps(self, e, reads, writes):
        deps = {}

        def add(tok):
            s, v = tok
            if deps.get(s, 0) < v:
                deps[s] = v

        for b in reads:
            if b.w is not None:
                if not (b.w[0] == e and e == "pe"):
                    add(b.w)
            if b.excl:
                for s, v in b.r.items():
                    if s != e:
                        add((s, v))
        for b in writes:
            if b.w is not None and not (b.w[0] == e and e == "pe"):
                add(b.w)
            for s, v in b.r.items():
                if not (s == e and e == "pe"):
                    add((s, v))
        return deps

    def _mark(self, tok, reads, writes):
        s, v = tok
        for b in reads:
            if b.r.get(s, 0) < v:
                b.r[s] = v
        for b in writes:
            b.w = tok
            b.r = {}

    def op(self, e, fn, reads=(), writes=(), inc=True):
        self._wait(e, self._deps(e, reads, writes))
        ins = fn(self.eng[e])
        self.n_ins += 1
        if inc:
            self.val[e] += 1
            ins.then_inc(self.sems[e], 1)
            tok = (e, self.val[e])
        else:
            tok = (e, self.val[e] + 1)
        self._mark(tok, reads, writes)
        return ins

    def dma(self, q, out, in_, reads=(), writes=(), chan="d0"):
        if chan not in self.sems:
            self._mk(chan)
        self._wait(q, self._deps(q, reads, writes))
        ins = self.eng[q].dma_start(out=out, in_=in_)
        self.val[chan] += 16
        ins.then_inc(self.sems[chan], 16)
        self.n_ins += 1
        self._mark((chan, self.val[chan]), reads, writes)
        self.chbufs.setdefault(chan, []).extend(writes)

    def seal(self, chan):
        for b in self.chbufs.get(chan, []):
            b.w = (chan, self.val[chan])

    def barrier(self):
        for e in self.eng:
            self._wait(e, dict(self.val))

    def finish(self):
        self._wait("sp", dict(self.val))


class TileT:
    def __init__(self, h, excl=False):
        self.h = h
        self.b = Buf(excl)

    def __getitem__(self, k):
        return self.h[k]


def sb(nc, name, shape, dt=F32):
    return TileT(nc.alloc_sbuf_tensor(name, list(shape), dt))


def ps(nc, name, shape, dt=F32):
    return TileT(nc.alloc_psum_tensor(name, list(shape), dt), True)


def build_E(do_mix, do_next):
    nc = bass.Bass("TRN2", target_bir_lowering=False)
    cx = Ctx(nc)
    NT = TC // TT
    xT = nc.dram_tensor("xT", [NDC, 128, TC], F32, kind="ExternalInput").ap()
    if do_mix:
        mixT = nc.dram_tensor("mixT", [NDC, 128, TC], BF16, kind="ExternalInput").ap()
        w_out = nc.dram_tensor("w_out", [NDC, 128, NDC * 128], F32, kind="ExternalInput").ap()
        w_up = nc.dram_tensor("w_up", [NFC, 128, NDC * 128], F32, kind="ExternalInput").ap()
        w_dn = nc.dram_tensor("w_dn", [NDC, 128, NFC * 128], F32, kind="ExternalInput").ap()
        g_mlp = nc.dram_tensor("g_mlp", [128, NDC], F32, kind="ExternalInput").ap()
        xo = nc.dram_tensor("xo", [NDC, 128, TC], F32, kind="ExternalOutput").ap()
    if do_next:
        g_nxt = nc.dram_tensor("g_nxt", [128, NDC], F32, kind="ExternalInput").ap()
        ho = nc.dram_tensor("ho", [NDC, 128, TC], BF16, kind="ExternalOutput").ap()

    x = sb(nc, "x", [128, NDC, TT])
    xb = [Buf() for _ in range(NDC)]
    h2 = sb(nc, "h2", [128, NDC, TT], BF16)
    h2b = [Buf() for _ in range(NDC)]
    sq = [sb(nc, f"sq{i}", [128, TT]) for i in range(2)]
    rt = sb(nc, "rt", [128, TT])
    rstd = sb(nc, "rstd", [128, TT])
    ones = sb(nc, "ones", [128, 128])
    epsb = sb(nc, "epsb", [128, 1])
    gm = sb(nc, "gm", [128, NDC])
    gn = sb(nc, "gn", [128, NDC])
    pacc = [ps(nc, f"pacc{i}", [128, TT]) for i in range(4)]
    pss = ps(nc, "pss", [128, TT])
    if do_mix:
        mix = sb(nc, "mix", [128, NDC, TT], BF16)
        u = sb(nc, "u", [128, NFC, TT], BF16)
        ub = [Buf() for _ in range(NFC)]
        rl = [sb(nc, f"rl{i}", [128, TT]) for i in range(2)]
        wo = [sb(nc, f"wo{i}", [128, NDC * 128], BF16) for i in range(2)]
        wu = [sb(nc, f"wu{i}", [128, NDC * 128], BF16) for i in range(3)]
        wd = [sb(nc, f"wd{i}", [128, 32 * 128], BF16) for i in range(3)]

    cx.op("dve", lambda e: e.memset(ones[:], 1.0), writes=[ones.b])
    cx.op("dve", lambda e: e.memset(epsb[:], RMS_EPS), writes=[epsb.b])
    if do_mix:
        cx.dma("sp", gm[:], g_mlp, writes=[gm.b], chan="dc")
    if do_next:
        cx.dma("sp", gn[:], g_nxt, writes=[gn.b], chan="dc")
    cx.seal("dc")

    cnt = {"acc": 0, "sq": 0, "wo": 0, "wu": 0, "wd": 0, "rl": 0}

    def rr(key, lst):
        i = cnt[key] % len(lst)
        cnt[key] += 1
        return lst[i]

    def rri(key, lst):
        i = cnt[key] % len(lst)
        cnt[key] += 1
        return lst[i], f"{key}{i}"

    def rms_to(dst, dstb, g):
        for dc in range(NDC):
            s = rr("sq", sq)
            cx.op("act", lambda e: e.activation(out=s[:], in_=x[:, dc, :], func=AF.Square),
                  reads=[xb[dc]], writes=[s.b])
            cx.op("pe", lambda e: e.matmul(pss[:], lhsT=ones[:], rhs=s[:], start=(dc == 0), stop=(dc == NDC - 1)),
                  reads=[ones.b, s.b], writes=[pss.b])
        cx.op("act", lambda e: e.activation(out=rt[:], in_=pss[:], func=AF.Sqrt, bias=epsb[:], scale=1.0 / D),
              reads=[pss.b, epsb.b], writes=[rt.b])
        cx.op("dve", lambda e: e.reciprocal(out=rstd[:], in_=rt[:]), reads=[rt.b], writes=[rstd.b])
        for dc in range(NDC):
            cx.op("dve", lambda e: e.scalar_tensor_tensor(out=dst[:, dc, :], in0=x[:, dc, :], scalar=g[:, dc:dc + 1],
                                                          in1=rstd[:], op0=ALU.mult, op1=ALU.mult),
                  reads=[xb[dc], g.b, rstd.b], writes=[dstb[dc]])

    for tt in range(NT):
        tsl = slice(tt * TT, (tt + 1) * TT)
        cx.dma("sp", x[:], xT[:, :, tsl].rearrange("c p t -> p c t"), writes=xb, chan="dx")
        if do_mix:
            cx.dma("sp", mix[:], mixT[:, :, tsl].rearrange("c p t -> p c t"), writes=[mix.b], chan="dm")
            for dc in range(NDC):
                w, ch = rri("wo", wo)
                cx.dma("pool", w[:], w_out[dc], writes=[w.b], chan=ch)
                pa = rr("acc", pacc)
                for kc in range(NDC):
                    cx.op("pe", lambda e: e.matmul(pa[:], lhsT=w[:, kc * 128:(kc + 1) * 128], rhs=mix[:, kc, :],
                                                   start=(kc == 0), stop=(kc == NDC - 1)),
                          reads=[w.b, mix.b], writes=[pa.b], inc=(kc == NDC - 1))
                cx.op("dve", lambda e: e.tensor_tensor(out=x[:, dc, :], in0=x[:, dc, :], in1=pa[:], op=ALU.add),
                      reads=[xb[dc], pa.b], writes=[xb[dc]])
            rms_to(h2, h2b, gm)
            for fc in range(NFC):
                w, ch = rri("wu", wu)
                cx.dma("pool", w[:], w_up[fc], writes=[w.b], chan=ch)
                pa = rr("acc", pacc)
                for dc in range(NDC):
                    cx.op("pe", lambda e: e.matmul(pa[:], lhsT=w[:, dc * 128:(dc + 1) * 128], rhs=h2[:, dc, :],
                                                   start=(dc == 0), stop=(dc == NDC - 1)),
                          reads=[w.b, h2b[dc]], writes=[pa.b], inc=(dc == NDC - 1))
                r = rr("rl", rl)
                cx.op("act", lambda e: e.activation(out=r[:], in_=pa[:], func=AF.Relu), reads=[pa.b], writes=[r.b])
                cx.op("dve", lambda e: e.tensor_tensor(out=u[:, fc, :], in0=r[:], in1=r[:], op=ALU.mult),
                      reads=[r.b], writes=[ub[fc]])
            for dc in range(NDC):
                pa = rr("acc", pacc)
                for hf in range(2):
                    w, ch = rri("wd", wd)
                    cx.dma("pool", w[:], w_dn[dc][:, hf * 4096:(hf + 1) * 4096], writes=[w.b], chan=ch)
                    for j in range(32):
                        fc = hf * 32 + j
                        cx.op("pe", lambda e: e.matmul(pa[:], lhsT=w[:, j * 128:(j + 1) * 128], rhs=u[:, fc, :],
                                                       start=(fc == 0), stop=(fc == NFC - 1)),
                              reads=[w.b, ub[fc]], writes=[pa.b], inc=(j == 31))
                cx.op("dve", lambda e: e.tensor_tensor(out=x[:, dc, :], in0=x[:, dc, :], in1=pa[:], op=ALU.add),
                      reads=[xb[dc], pa.b], writes=[xb[dc]])
            cx.dma("sp", xo[:, :, tsl].rearrange("c p t -> p c t"), x[:], reads=xb, chan="sx")
        if do_next:
            rms_to(h2, h2b, gn)
            cx.dma("sp", ho[:, :, tsl].rearrange("c p t -> p c t"), h2[:], reads=h2b, chan="sh")
    cx.finish()
    return nc


NTT = T // TT
NKT = T // 128
BOFF = 4
ATT_COLS = 5 * 64 + 64
SLOPES = [2.0 ** (-(i + 1)) for i in range(8)]


def alibi_tables(head):
    sl = SLOPES[head]
    kaug = np.zeros((3, T), np.float32)
    j = np.arange(T)
    kaug[0] = sl * (j % 128)
    kaug[1] = 1.0
    kaug[2] = 1.0
    qaug = np.zeros((3, TT), np.float32)
    i = np.arange(TT)
    qaug[0] = 1.0
    qaug[1] = -sl * (i % 128)
    qaug[2] = -sl * 128 * (i // 128)
    btbl = np.zeros((128, 128 + BOFF), np.float32)
    for m in range(-BOFF, 128):
        btbl[:, m + BOFF] = -sl * 128.0 * m
    jj = np.arange(128)[:, None]
    bb = np.arange(128)[None, :]
    cm = np.where(jj > bb, -2.0 * sl * (jj - bb), 0.0) + np.where((jj // 64) <= (bb // 64), 0.0, -30000.0)
    return (kaug.astype(ml_dtypes.bfloat16), qaug.astype(ml_dtypes.bfloat16), btbl, cm.astype(np.float32))


def emit_attn(nc, cx, es, l, hT, w_att, prm_a, kaug_d, qaug_d, btbl_d, cmat_d, mix_out, ntiles=NTT):
    lam_init = 0.8 - 0.6 * math.exp(-0.3 * l)

    def S(name, shape, dt=F32):
        return TileT(es.enter_context(nc.sbuf_tensor("sA_" + name, list(shape), dt)))

    def P(name, shape, dt=F32):
        return TileT(es.enter_context(nc.psum_tensor("pA_" + name, list(shape), dt)), True)

    K = [S(f"K{m}", [67, T], BF16) for m in range(2)]
    Kb = [[Buf() for _ in range(NKT)] for m in range(2)]
    V = S("V", [128, NKT, 130], BF16)
    Vb = [Buf() for _ in range(NKT)]
    W = S("Watt", [128, NDC, ATT_COLS], BF16)
    h = S("hA", [128, NDC, TT], BF16)
    qa = [[S(f"qa{m}_{i}", [67, TT], BF16) for i in range(2)] for m in range(2)]
    prm = S("prmA", [128, 8])
    gsub = S("gsub", [128, 128])
    lqk = S("lqk", [1, 4, 64])
    btbl = S("btbl", [128, 128 + BOFF])
    cmat = S("cmat", [128, 128])
    ones = S("onesA", [128, 128])
    identb = S("identb", [128, 128], BF16)
    identf = S("identf", [128, 128])
    eps = S("epsA", [128, 2])
    lam = S("lam", [128, 1])
    sqt = S("sqt", [64, 4, TT])
    rtt = S("rtt", [64, 4, TT])
    rst = S("rst", [64, 4, TT])
    pt = [S(f"pt{i}", [128, TT], BF16) for i in range(4)]
    dg = [S(f"dg{i}", [128, 128]) for i in range(2)]
    fin = {k: S("fin_" + k, s, d) for k, s, d in [("r1", [128, 1], F32), ("r2", [128, 1], F32), ("t2", [128, 128], F32),
                                                  ("t", [128, 128], F32), ("sq", [128, 128], F32), ("ss", [128, 1], F32),
                                                  ("rt", [128, 1], F32), ("rs", [128, 1], F32), ("an", [128, 128], BF16)]}
    mo = [S(f"mo{i}", [128, TT], BF16) for i in range(2)]
    tiny = S("tiny", [1, 8])
    pq = [P(f"pq{i}", [128, TT]) for i in range(3)]
    po = [[P(f"po{m}_{i}", [128, 2, 256]) for i in range(2)] for m in range(2)]
    pT = P("pT", [128, 1024], BF16)
    pob = [[[po[m][i].b, po[m][i].b] for i in range(2)] for m in range(2)]

    cx.op("dve", lambda e: e.memset(ones[:], 1.0), writes=[ones.b])
    cx.op("dve", lambda e: e.memset(eps[:, 0:1], 64.0 * RMS_EPS), writes=[eps.b])
    cx.op("dve", lambda e: e.memset(eps[:, 1:2], RMS_EPS), writes=[eps.b])
    cx.op("pool", lambda e: e.memset(identf[:], 1.0), writes=[identf.b])
    cx.op("pool", lambda e: e.affine_select(out=identf[:], in_=identf[:], pattern=[[-1, 128]], compare_op=ALU.is_equal,
                                            fill=0.0, base=0, channel_multiplier=1), reads=[identf.b], writes=[identf.b])
    cx.op("dve", lambda e: e.tensor_copy(out=identb[:], in_=identf[:]), reads=[identf.b], writes=[identb.b])
    cx.op("dve", lambda e: e.memset(V[:], 1.0), writes=Vb)
    cx.dma("sp", prm[:], prm_a["prm"], writes=[prm.b], chan="dc")
    cx.dma("sp", gsub[:], prm_a["gsub"], writes=[gsub.b], chan="dc")
    cx.dma("sp", lqk[:], prm_a["lqk"], writes=[lqk.b], chan="dc")
    cx.dma("sp", btbl[:], btbl_d, writes=[btbl.b], chan="dc")
    cx.dma("sp", cmat[:], cmat_d, writes=[cmat.b], chan="dc")
    for m in range(2):
        cx.dma("sp", K[m][64:67, :], kaug_d, writes=Kb[m], chan="dc")
        for i in range(2):
            cx.dma("sp", qa[m][i][64:67, :], qaug_d, writes=[qa[m][i].b], chan="dc")
    cx.seal("dc")
    cx.dma("pool", W[:], w_att, writes=[W.b], chan="dW")
    cx.op("dve", lambda e: e.tensor_tensor(out=lqk[:, 0:2, :], in0=lqk[:, 0:2, :], in1=lqk[:, 2:4, :], op=ALU.mult),
          reads=[lqk.b], writes=[lqk.b])
    cx.op("dve", lambda e: e.tensor_reduce(out=tiny[:, 0:2], in_=lqk[:, 0:2, :], axis=mybir.AxisListType.X, op=ALU.add),
          reads=[lqk.b], writes=[tiny.b])
    cx.op("act", lambda e: e.activation(out=tiny[:, 2:4], in_=tiny[:, 0:2], func=AF.Exp), reads=[tiny.b], writes=[tiny.b])
    cx.op("dve", lambda e: e.tensor_tensor(out=tiny[:, 4:5], in0=tiny[:, 2:3], in1=tiny[:, 3:4], op=ALU.subtract),
          reads=[tiny.b], writes=[tiny.b])
    cx.op("dve", lambda e: e.tensor_scalar(out=tiny[:, 5:6], in0=tiny[:, 4:5], scalar1=prm[0:1, 4:5], scalar2=None, op0=ALU.add),
          reads=[tiny.b, prm.b], writes=[tiny.b])
    cx.op("pe", lambda e: e.matmul(pq[0][:, 0:1], lhsT=ones[0:1, :], rhs=tiny[:, 5:6], start=True, stop=True),
          reads=[ones.b, tiny.b], writes=[pq[0].b])
    cx.op("dve", lambda e: e.tensor_copy(out=lam[:], in_=pq[0][:, 0:1]), reads=[pq[0].b], writes=[lam.b])
    cx.op("dve", lambda e: e.tensor_scalar(out=gsub[:], in0=gsub[:], scalar1=prm[:, 5:6], scalar2=None, op0=ALU.mult),
          reads=[gsub.b, prm.b], writes=[gsub.b])

    cnt = {"pq": 0, "pt": 0, "dg": 0}

    def rr(key, lst):
        i = cnt[key] % len(lst)
        cnt[key] += 1
        return lst[i]

    import os
    STOP = int(os.environ.get("DBG_STOP", "99"))
    SUB = int(os.environ.get("DBG_SUB", "99"))
    if STOP < 1:
        return
    for s in range(ntiles):
        tsl = slice(s * TT, (s + 1) * TT)
        cx.dma("sp", h[:], hT[:, :, tsl].rearrange("c p t -> p c t"), writes=[h.b], chan="dh")
        q = [qa[0][s % 2], qa[1][s % 2]]
        pg = [rr("pq", pq) for _ in range(4)]
        for g in range(4):
            for dc in range(NDC):
                cx.op("pe", lambda e: e.matmul(pg[g][0:64, :], lhsT=W[:, dc, g * 64:(g + 1) * 64], rhs=h[:, dc, :],
                                               start=(dc == 0), stop=(dc == NDC - 1)),
                      reads=[W.b, h.b], writes=[pg[g].b], inc=(dc == NDC - 1))
            if SUB < 2:
                continue
            cx.op("act", lambda e: e.activation(out=sqt[:, g, :], in_=pg[g][0:64, :], func=AF.Square),
                  reads=[pg[g].b], writes=[sqt.b])
            cx.op("dve", lambda e: e.tensor_copy(out=rtt[:, g, :], in_=pg[g][0:64, :]), reads=[pg[g].b], writes=[rtt.b])
        if SUB < 3:
            continue
        for g in range(4):
            cx.op("pe", lambda e: e.matmul(pg[g][0:64, :], lhsT=ones[0:64, 0:64], rhs=sqt[:, g, :], start=True, stop=True),
                  reads=[ones.b, sqt.b, rtt.b], writes=[pg[g].b])
            if SUB < 4:
                continue
            if g < 2:
                cx.op("act", lambda e: e.activation(out=sqt[:, g, :], in_=pg[g][0:64, :], func=AF.Sqrt, bias=eps[0:64, 0:1],
                                                    scale=1.0), reads=[pg[g].b, eps.b], writes=[sqt.b])
            else:
                cx.op("act", lambda e: e.activation(out=sqt[:, g, :], in_=pg[g][0:64, :], func=AF.Sqrt, bias=eps[0:64, 1:2],
                                                    scale=1.0 / 64), reads=[pg[g].b, eps.b], writes=[sqt.b])
        if SUB < 5:
            continue
        cx.op("dve", lambda e: e.reciprocal(out=rst[:], in_=sqt[:]), reads=[sqt.b], writes=[rst.b])
        if SUB < 6:
            continue
        for g in range(4):
            if g < 2:
                dst, dstb = q[g][0:64, :], q[g].b
            else:
                dst, dstb = K[g - 2][0:64, tsl], None
            wr = [dstb] if dstb is not None else Kb[g - 2][4 * s:4 * s + 4]
            cx.op("dve", lambda e: e.scalar_tensor_tensor(out=dst, in0=rtt[:, g, :], scalar=prm[0:64, g:g + 1], in1=rst[:, g, :],
                                                          op0=ALU.mult, op1=ALU.mult),
                  reads=[rtt.b, prm.b, rst.b], writes=wr)
        if STOP < 2:
            continue
        for sub in range(4):
            pv = rr("pq", pq)
            for dc in range(NDC):
                cx.op("pe", lambda e: e.matmul(pv[:, 0:128], lhsT=h[:, dc, sub * 128:(sub + 1) * 128], rhs=W[:, dc, 256:384],
                                               start=(dc == 0), stop=(dc == NDC - 1)),
                      reads=[W.b, h.b], writes=[pv.b], inc=(dc == NDC - 1))
            cx.op("act", lambda e: e.activation(out=V[:, 4 * s + sub, 0:128], in_=pv[:, 0:128], func=AF.Copy),
                  reads=[pv.b], writes=[Vb[4 * s + sub]])
        if STOP < 3:
            continue
        nk = 4 * s + 4
        units = [(kt, m) for kt in range(nk) for m in range(2)]

        def qk(kt, m):
            pS = rr("pq", pq)
            p = rr("pt", pt)
            mm = kt - 4 * s
            c0 = 128 * mm if mm > 0 else 0
            cx.op("pe", lambda e: e.matmul(pS[:, c0:TT], lhsT=K[m][0:67, kt * 128:(kt + 1) * 128], rhs=q[m][0:67, c0:TT],
                                           start=True, stop=True), reads=[Kb[m][kt], q[m].b], writes=[pS.b])
            bcol = BOFF + (4 * s - kt)
            if mm >= 0:
                d = rr("dg", dg)
                cx.op("dve", lambda e: e.tensor_tensor(out=d[:], in0=pS[:, c0:c0 + 128], in1=cmat[:], op=ALU.add),
                      reads=[pS.b, cmat.b], writes=[d.b])
                cx.op("act", lambda e: e.activation(out=p[:, c0:c0 + 128], in_=d[:], func=AF.Exp, bias=btbl[:, bcol:bcol + 1],
                                                    scale=1.0), reads=[d.b, btbl.b], writes=[p.b])
                if c0 + 128 < TT:
                    cx.op("act", lambda e: e.activation(out=p[:, c0 + 128:TT], in_=pS[:, c0 + 128:TT], func=AF.Exp,
                                                        bias=btbl[:, bcol:bcol + 1], scale=1.0),
                          reads=[pS.b, btbl.b], writes=[p.b])
            else:
                cx.op("act", lambda e: e.activation(out=p[:], in_=pS[:], func=AF.Exp, bias=btbl[:, bcol:bcol + 1], scale=1.0),
                      reads=[pS.b, btbl.b], writes=[p.b])
            return p

        def pvmm(kt, m, p):
            mm = kt - 4 * s
            a0 = mm if mm > 0 else 0
            for a in range(a0, 4):
                cx.op("pe", lambda e: e.matmul(po[m][a // 2][:, a % 2, 0:129], lhsT=p[:, a * 128:(a + 1) * 128],
                                               rhs=V[:, kt, 0:129], start=(kt == 0 and a % 2 == 0), stop=(kt == 4 * s + a),
                                               skip_group_check=True),
                      reads=[p.b, Vb[kt]], writes=[pob[m][a // 2][a % 2]], inc=(a == 3))

        prev = None
        for (kt, m) in units:
            p = qk(kt, m)
            if prev is not None:
                pvmm(*prev)
            prev = (kt, m, p)
        pvmm(*prev)
        if STOP < 4:
            continue
        mo_t = mo[s % 2]
        for a in range(4):
            o1 = po[0][a // 2]
            o2 = po[1][a // 2]
            b1 = pob[0][a // 2][a % 2]
            b2 = pob[1][a // 2][a % 2]
            f = fin
            cx.op("dve", lambda e: e.reciprocal(out=f["r1"][:], in_=o1[:, a % 2, 128:129]), reads=[b1], writes=[f["r1"].b])
            cx.op("dve", lambda e: e.reciprocal(out=f["r2"][:], in_=o2[:, a % 2, 128:129]), reads=[b2], writes=[f["r2"].b])
            cx.op("dve", lambda e: e.tensor_tensor(out=f["r2"][:], in0=f["r2"][:], in1=lam[:], op=ALU.mult),
                  reads=[f["r2"].b, lam.b], writes=[f["r2"].b])
            cx.op("dve", lambda e: e.tensor_scalar(out=f["t2"][:], in0=o2[:, a % 2, 0:128], scalar1=f["r2"][:, 0:1], scalar2=None,
                                                   op0=ALU.mult), reads=[b2, f["r2"].b], writes=[f["t2"].b])
            cx.op("dve", lambda e: e.scalar_tensor_tensor(out=f["t"][:], in0=o1[:, a % 2, 0:128], scalar=f["r1"][:, 0:1],
                                                          in1=f["t2"][:], op0=ALU.mult, op1=ALU.subtract),
                  reads=[b1, f["r1"].b, f["t2"].b], writes=[f["t"].b])
            cx.op("act", lambda e: e.activation(out=f["sq"][:], in_=f["t"][:], func=AF.Square, accum_out=f["ss"][:]),
                  reads=[f["t"].b], writes=[f["sq"].b, f["ss"].b])
            cx.op("act", lambda e: e.activation(out=f["rt"][:], in_=f["ss"][:], func=AF.Sqrt, bias=eps[:, 1:2], scale=1.0 / 128),
                  reads=[f["ss"].b, eps.b], writes=[f["rt"].b])
            cx.op("dve", lambda e: e.reciprocal(out=f["rs"][:], in_=f["rt"][:]), reads=[f["rt"].b], writes=[f["rs"].b])
            cx.op("dve", lambda e: e.scalar_tensor_tensor(out=f["an"][:], in0=f["t"][:], scalar=f["rs"][:, 0:1], in1=gsub[:],
                                                          op0=ALU.mult, op1=ALU.mult),
                  reads=[f["t"].b, f["rs"].b, gsub.b], writes=[f["an"].b])
            cx.op("pe", lambda e: e.transpose(out=pT[:, 0:128], in_=f["an"][:], identity=identb[:]),
                  reads=[f["an"].b, identb.b], writes=[pT.b])
            cx.op("act", lambda e: e.activation(out=mo_t[:, a * 128:(a + 1) * 128], in_=pT[:, 0:128], func=AF.Copy),
                  reads=[pT.b], writes=[mo_t.b])
        cx.dma("sp", mix_out[0:128, tsl], mo_t[:], reads=[mo_t.b], chan=f"mo{s % 2}")


def build_M(l, do_attn=True, do_rwkv=True, ntiles=NTT):
    from contextlib import ExitStack
    nc = bass.Bass("TRN2", target_bir_lowering=False)
    cx = Ctx(nc)
    NTK = ntiles * TT
    hT = nc.dram_tensor("hT", [NDC, 128, NTK], BF16, kind="ExternalInput").ap()
    mix_out = nc.dram_tensor("mix_out", [256, NTK], BF16, kind="ExternalOutput").ap()

    def din(name, shape, dt=F32):
        return nc.dram_tensor(name, list(shape), dt, kind="ExternalInput").ap()

    if do_attn:
        w_att = din("w_att", [128, NDC, ATT_COLS])
        prm_a = {"prm": din("prm_a", [128, 8]), "gsub": din("gsub", [128, 128]), "lqk": din("lqk", [1, 4, 64])}
        kaug = din("kaug", [3, T], BF16)
        qaug = din("qaug", [3, TT], BF16)
        btbl = din("btbl", [128, 128 + BOFF])
        cmat = din("cmat", [128, 128])
        with ExitStack() as es:
            emit_attn(nc, cx, es, l, hT, w_att, prm_a, kaug, qaug, btbl, cmat, mix_out, ntiles)
            cx.barrier()
    if do_rwkv:
        l = 1
        nch = 14
        d = {"w_r": din("w_r", [nch, 128, NDC, 128]), "prm_r": din("prm_r", [128, NPR]), "w2a2": din("w2a2", [128, 128]),
             "g2": din("g2", [160, 128]), "msl": din("msl", [128, 128]), "mslT": din("mslT", [128, 128]),
             "mil": din("mil", [128, 128]), "bones": din("bones", [128, 128]), "scanm": din("scanm", [128, TT])}
        d["v1"] = din("v1", [128, 8, 32])
        d["v2"] = din("v2", [32, 128])
        d["vfirst"] = din("vfirst", [128, NTK])
        d["vfirst_o"] = nc.dram_tensor("vfirst_o", [128, NTK], F32, kind="ExternalOutput").ap()
        with ExitStack() as es:
            emit_rwkv(nc, cx, es, l, hT, d, mix_out, ntiles)
            cx.barrier()
    cx.finish()
    return nc


def rwkv_inputs(inp, l, c):
    w = inp["w_in"][l]
    own = np.arange(c * 128, c * 128 + 128)
    base = 3072
    chunks = [base + own, base + 1024 + own, base + 2048 + own, base + 3072 + np.arange(128)]
    chunks.append(base + 3072 + 128 + np.arange(128))
    g1 = np.full(128, -1)
    g1[0:32] = base + 3072 + 256 + np.arange(32)
    chunks.append(g1)
    for j in range(8):
        chunks.append(base + 2048 + j * 128 + np.arange(128))
    w_r = np.zeros((len(chunks), 128, NDC, 128), np.float32)
    for i, cols in enumerate(chunks):
        valid = cols >= 0
        blk = np.zeros((D, 128), np.float32)
        blk[:, valid] = w[:, cols[valid]]
        w_r[i] = blk.reshape(NDC, 128, 128).transpose(1, 0, 2)
    mu = inp["rwkv_mu"][l]
    prm = np.zeros((128, NPR), np.float32)
    prm[:, 0] = mu[own]
    prm[:, 1] = mu[1024 + own]
    prm[:, 2] = mu[2048 + own]
    prm[:, 3] = mu[3072:3072 + 128]
    prm[:, 4] = mu[3072 + 128:3072 + 256]
    prm[0:32, 5] = mu[3072 + 256:3072 + 288]
    prm[:, 6] = inp["rwkv_w0"][l][own]
    prm[:, 7] = inp["rwkv_a0"][l][own]
    prm[:, 8] = inp["rwkv_k_k"][l][own]
    prm[:, 9] = inp["rwkv_k_a"][l][own]
    prm[:, 10] = inp["rwkv_r_k"][l].reshape(-1)[own]
    prm[:, 11] = inp["rwkv_ln_w"][l][own]
    prm[:, 12] = inp["rwkv_ln_b"][l][own]
    out = {"w_r": w_r, "w2a2": np.concatenate([inp["rwkv_w2"][l][:, own], inp["rwkv_a2"][l][:, own]], 0).astype(np.float32),
           "g2": np.ascontiguousarray(inp["rwkv_g2"][l][:, own])}
    for j in range(8):
        prm[:, 14 + j] = mu[2048 + j * 128:2048 + (j + 1) * 128]
    if l > 0:
        prm[:, 13] = inp["rwkv_v0"][l - 1][own]
        out["v1"] = np.ascontiguousarray(inp["rwkv_v1"][l - 1].reshape(8, 128, 32).transpose(1, 0, 2))
        out["v2"] = np.ascontiguousarray(inp["rwkv_v2"][l - 1][:, own])
    else:
        prm[:, 13] = -30000.0
        out["v1"] = np.zeros((128, 8, 32), np.float32)
        out["v2"] = np.zeros((32, 128), np.float32)
    out["prm_r"] = prm
    out.update(rwkv_consts())
    return out


def attn_inputs(inp, l, c):
    w = inp["w_in"][l]
    cols = np.concatenate([np.arange(c * 128, c * 128 + 128), 1024 + np.arange(c * 128, c * 128 + 128),
                           2048 + np.arange(c * 128, c * 128 + 128)])
    w_att = np.ascontiguousarray(w[:, cols].reshape(NDC, 128, ATT_COLS).transpose(1, 0, 2))
    prm = np.zeros((128, 8), np.float32)
    prm[0:64, 0] = inp["qk_norm_q"][l][0]
    prm[0:64, 1] = inp["qk_norm_q"][l][1]
    prm[0:64, 2] = inp["qk_norm_k"][l][0]
    prm[0:64, 3] = inp["qk_norm_k"][l][1]
    lam_init = 0.8 - 0.6 * math.exp(-0.3 * l)
    prm[:, 4] = lam_init
    prm[:, 5] = 1.0 - lam_init
    gsub = np.ascontiguousarray(np.broadcast_to(inp["diff_subln"][l][None, :], (128, 128))).astype(np.float32)
    lqk = np.stack([inp["diff_lambda_q"][l][0], inp["diff_lambda_q"][l][1], inp["diff_lambda_k"][l][0],
                    inp["diff_lambda_k"][l][1]])[None].astype(np.float32)
    kaug, qaug, btbl, cmat = alibi_tables(c)
    return {"w_att": w_att, "prm_a": prm, "gsub": gsub, "lqk": lqk, "kaug": kaug, "qaug": qaug, "btbl": btbl, "cmat": cmat}


C0 = math.exp(-0.5)
NPR = 24


def rwkv_consts():
    hh = np.arange(128) // 64
    tt = np.arange(128) % 64
    same = hh[:, None] == hh[None, :]
    msl = (same & (tt[:, None] < tt[None, :])).astype(np.float32)
    mslT = np.ascontiguousarray(msl.T)
    mil = (same & (tt[:, None] <= tt[None, :])).astype(np.float32)
    bones = same.astype(np.float32)
    scanm = np.ones((128, TT), np.float32)
    scanm[:, ::64] = 0.0
    return {"msl": msl, "mslT": mslT, "mil": mil, "bones": bones, "scanm": scanm}


def emit_rwkv(nc, cx, es, l, hT, d, mix_out, ntiles=NTT):
    nch = 6 + (8 if l > 0 else 0)

    def S(name, shape, dt=F32):
        return TileT(es.enter_context(nc.sbuf_tensor("sR_" + name, list(shape), dt)))

    def P(name, shape, dt=F32):
        return TileT(es.enter_context(nc.psum_tensor("pR_" + name, list(shape), dt)), True)

    h = S("h", [128, NDC, TT], BF16)
    wr = [S(f"wr{i}", [128, NDC * 128], BF16) for i in range(2)]
    prm = S("prm", [128, NPR])
    w2a2 = S("w2a2", [128, 128], BF16)
    g2t = S("g2t", [128, 2, 128], BF16)
    msl = S("msl", [128, 128]); mslT = S("mslT", [128, 128]); mil = S("mil", [128, 128])
    bones = S("bones", [128, 128]); scanm = S("scanm", [128, TT])
    identf = S("identf", [128, 128]); identb = S("identb", [128, 128], BF16)
    cst = S("cst", [128, 4])
    raw = {q: S("raw_" + q, [128, TT + 1]) for q in ["r", "k", "v", "wa", "g0", "g1"]}
    sh = {q: S("sh_" + q, [128, TT]) for q in ["r", "k", "v", "wa", "g0", "g1"]}
    tmp = [S(f"tmp{i}", [128, TT]) for i in range(3)]
    wab = S("wab", [128, TT], BF16)
    sg0 = S("sg0", [128, TT], BF16); sg1 = S("sg1", [32, TT], BF16)
    sig = S("sig", [128, TT]); arate = S("arate", [128, TT]); gout = S("gout", [128, TT])
    kk = S("kk", [128, TT]); kmod = S("kmod", [128, TT]); bonus = S("bonus", [128, TT])
    cs = S("cs", [128, TT]); epos = S("epos", [128, TT]); eneg = S("eneg", [128, TT]); eprev = S("eprev", [128, TT])
    ec = S("ec", [128, 8]); bneg = S("bneg", [128, TT]); kneg = S("kneg", [128, TT])
    ysc = S("ysc", [128, TT]); yc = S("yc", [128, TT])
    bd = {q: S("bd_" + q, [128, 8, 128], BF16) for q in ["a", "b", "k", "r", "v", "bh", "kh"]}
    mo = [S(f"mo{i}", [128, TT], BF16) for i in range(2)]
    Hb = S("Hb", [128, 128], BF16)
    def cset(i):
        t = {}
        for n in ["LT", "L", "AkT", "ArbT", "ArkT", "MT", "RbT", "P1", "PT1", "P2", "PT2"]:
            t[n] = S(f"c{i}_{n}", [128, 128], BF16)
        t["tok"] = S(f"c{i}_tok", [128, 4, 128], BF16)
        t["X"] = S(f"c{i}_X", [128, 256])
        t["Xb"] = S(f"c{i}_Xb", [128, 256], BF16)
        t["Nc"] = S(f"c{i}_Nc", [128, 128])
        return t
    cs_ = [cset(0), cset(1)]
    if l > 0:
        v1t = S("v1t", [128, 8, 32], BF16); v2t = S("v2t", [32, 128], BF16)
        cvf = S("cvf", [128, 8]); rawx = S("rawx", [128, TT + 1])
        vfsh = S("vfsh", [128, 8, TT], BF16); lvb = S("lvb", [32, TT], BF16)
        vft = S("vft", [128, TT]); gate = S("gate", [128, TT])
    pp = [P(f"p{i}", [128, TT]) for i in range(7)]
    ptb = P("ptb", [128, 1024], BF16)

    cnt = {"pp": 0, "wr": 0}

    def rr(key, lst):
        i = cnt[key] % len(lst)
        cnt[key] += 1
        return lst[i]

    def rri(key, lst):
        i = cnt[key] % len(lst)
        cnt[key] += 1
        return lst[i], f"R{key}{i}"

    for name, t in [("msl", msl), ("mslT", mslT), ("mil", mil), ("bones", bones), ("scanm", scanm), ("prm_r", prm)]:
        cx.dma("sp", t[:], d[name], writes=[t.b], chan="dcR")
    cx.seal("dcR")
    cx.dma("pool", w2a2[:], d["w2a2"], writes=[w2a2.b], chan="dcR2")
    cx.dma("pool", g2t[:, 0, :], d["g2"][0:128, :], writes=[g2t.b], chan="dcR2")
    cx.dma("pool", g2t[0:32, 1, :], d["g2"][128:160, :], writes=[g2t.b], chan="dcR2")
    if l > 0:
        cx.dma("pool", v1t[:], d["v1"], writes=[v1t.b], chan="dcR2")
        cx.dma("pool", v2t[:], d["v2"], writes=[v2t.b], chan="dcR2")
        cx.op("dve", lambda e: e.memset(cvf[:], 0.0), writes=[cvf.b])
    cx.seal("dcR2")
    cx.op("pool", lambda e: e.memset(identf[:], 1.0), writes=[identf.b])
    cx.op("pool", lambda e: e.affine_select(out=identf[:], in_=identf[:], pattern=[[-1, 128]], compare_op=ALU.is_equal,
                                            fill=0.0, base=0, channel_multiplier=1), reads=[identf.b], writes=[identf.b])
    cx.op("dve", lambda e: e.tensor_copy(out=identb[:], in_=identf[:]), reads=[identf.b], writes=[identb.b])
    cx.op("dve", lambda e: e.memset(cst[:, 0:1], GN_EPS), writes=[cst.b])
    cx.op("dve", lambda e: e.memset(cst[:, 1:2], 0.0), writes=[cst.b])
    cx.op("dve", lambda e: e.memset(Hb[:], 0.0), writes=[Hb.b])
    for q in bd:
        cx.op("pool", lambda e: e.memset(bd[q][:], 0.0), writes=[bd[q].b])
    for q in raw:
        cx.op("dve", lambda e: e.memset(raw[q][:], 0.0), writes=[raw[q].b])

    def c3(t):
        return t.h[:, :].rearrange("p (c t) -> p c t", t=64)

    HALF = [(slice(0, 64), slice(0, 64)), (slice(64, 128), slice(64, 128))]

    for s in range(ntiles):
        tsl = slice(s * TT, (s + 1) * TT)
        cx.dma("sp", h[:], hT[:, :, tsl].rearrange("c p t -> p c t"), writes=[h.b], chan="dhR")

        def inproj(ch, M):
            w, chn = rri("wr", wr)
            cx.dma("pool", w[:], d["w_r"][ch].rearrange("p c j -> p (c j)"), writes=[w.b], chan=chn)
            p = rr("pp", pp)
            for dc in range(NDC):
                cx.op("pe", lambda e: e.matmul(p[0:M, :], lhsT=w[:, dc * 128:dc * 128 + M], rhs=h[:, dc, :],
                                               start=(dc == 0), stop=(dc == NDC - 1)),
                      reads=[w.b, h.b], writes=[p.b], inc=(dc == NDC - 1))
            return p

        def shift(p, rw, out_ap, outb, mu_ap, M=128):
            cx.op("act", lambda e: e.activation(out=rw[0:M, 1:TT + 1], in_=p[0:M, :], func=AF.Copy), reads=[p.b], writes=[rw.b])
            t0 = tmp[0]
            cx.op("dve", lambda e: e.tensor_tensor(out=t0[0:M, :], in0=rw[0:M, 0:TT], in1=rw[0:M, 1:TT + 1], op=ALU.subtract),
                  reads=[rw.b], writes=[t0.b])
            cx.op("dve", lambda e: e.scalar_tensor_tensor(out=out_ap, in0=t0[0:M, :], scalar=mu_ap, in1=rw[0:M, 1:TT + 1],
                                                          op0=ALU.mult, op1=ALU.add), reads=[t0.b, rw.b, prm.b], writes=[outb])

        for qi, q in enumerate(["r", "k", "v", "wa", "g0", "g1"]):
            M = 32 if q == "g1" else 128
            p = inproj(qi, M)
            rw = raw[q]
            shift(p, rw, sh[q][0:M, :], sh[q].b, prm[0:M, qi:qi + 1], M)
            cx.op("dve", lambda e: e.tensor_copy(out=rw[0:M, 0:1], in_=rw[0:M, TT:TT + 1]), reads=[rw.b], writes=[rw.b])
        v = sh["v"]
        if l > 0:
            for j in range(8):
                p = inproj(6 + j, 128)
                cx.op("dve", lambda e: e.tensor_copy(out=rawx[:, 0:1], in_=cvf[:, j:j + 1]), reads=[cvf.b], writes=[rawx.b])
                shift(p, rawx, vfsh[:, j, :], vfsh.b, prm[:, 14 + j:15 + j])
                cx.op("dve", lambda e: e.tensor_copy(out=cvf[:, j:j + 1], in_=rawx[:, TT:TT + 1]), reads=[rawx.b], writes=[cvf.b])
            p = rr("pp", pp)
            for j in range(8):
                cx.op("pe", lambda e: e.matmul(p[0:32, :], lhsT=v1t[:, j, :], rhs=vfsh[:, j, :], start=(j == 0), stop=(j == 7)),
                      reads=[v1t.b, vfsh.b], writes=[p.b], inc=(j == 7))
            cx.op("act", lambda e: e.activation(out=lvb[:], in_=p[0:32, :], func=AF.Copy), reads=[p.b], writes=[lvb.b])
            p = rr("pp", pp)
            cx.op("pe", lambda e: e.matmul(p[:], lhsT=v2t[:], rhs=lvb[:], start=True, stop=True), reads=[v2t.b, lvb.b], writes=[p.b])
            cx.op("act", lambda e: e.activation(out=gate[:], in_=p[:], func=AF.Sigmoid, bias=prm[:, 13:14], scale=1.0),
                  reads=[p.b, prm.b], writes=[gate.b])
            cx.dma("sp", vft[:], d["vfirst"][:, tsl], writes=[vft.b], chan="dvf")
            cx.op("dve", lambda e: e.tensor_tensor(out=vft[:], in0=vft[:], in1=v[:], op=ALU.subtract), reads=[vft.b, v.b], writes=[vft.b])
            cx.op("dve", lambda e: e.tensor_tensor(out=vft[:], in0=vft[:], in1=gate[:], op=ALU.mult), reads=[vft.b, gate.b], writes=[vft.b])
            cx.op("dve", lambda e: e.tensor_tensor(out=v[:], in0=v[:], in1=vft[:], op=ALU.add), reads=[vft.b, v.b], writes=[v.b])
        cx.dma("sp", d["vfirst_o"][:, tsl], v[:], reads=[v.b], chan="svf")
        cx.op("act", lambda e: e.activation(out=wab[0:64, :], in_=sh["wa"][0:64, :], func=AF.Tanh), reads=[sh["wa"].b], writes=[wab.b])
        cx.op("dve", lambda e: e.tensor_copy(out=wab[64:128, :], in_=sh["wa"][64:128, :]), reads=[sh["wa"].b], writes=[wab.b])
        p = rr("pp", pp)
        cx.op("pe", lambda e: e.matmul(p[:], lhsT=w2a2[0:64, :], rhs=wab[0:64, :], start=True, stop=True), reads=[w2a2.b, wab.b], writes=[p.b])
        cx.op("act", lambda e: e.activation(out=sig[:], in_=p[:], func=AF.Sigmoid, bias=prm[:, 6:7], scale=1.0),
              reads=[p.b, prm.b], writes=[sig.b])
        p = rr("pp", pp)
        cx.op("pe", lambda e: e.matmul(p[:], lhsT=w2a2[64:128, :], rhs=wab[64:128, :], start=True, stop=True), reads=[w2a2.b, wab.b], writes=[p.b])
        cx.op("act", lambda e: e.activation(out=arate[:], in_=p[:], func=AF.Sigmoid, bias=prm[:, 7:8], scale=1.0),
              reads=[p.b, prm.b], writes=[arate.b])
        cx.op("act", lambda e: e.activation(out=sg0[:], in_=sh["g0"][:], func=AF.Sigmoid), reads=[sh["g0"].b], writes=[sg0.b])
        cx.op("act", lambda e: e.activation(out=sg1[:], in_=sh["g1"][0:32, :], func=AF.Sigmoid), reads=[sh["g1"].b], writes=[sg1.b])
        p = rr("pp", pp)
        cx.op("pe", lambda e: e.matmul(p[:], lhsT=g2t[:, 0, :], rhs=sg0[:], start=True, stop=False), reads=[g2t.b, sg0.b], writes=[p.b], inc=False)
        cx.op("pe", lambda e: e.matmul(p[:], lhsT=g2t[0:32, 1, :], rhs=sg1[:], start=False, stop=True), reads=[g2t.b, sg1.b], writes=[p.b])
        cx.op("act", lambda e: e.activation(out=gout[:], in_=p[:], func=AF.Copy), reads=[p.b], writes=[gout.b])
        t1, t2 = tmp[1], tmp[2]
        cx.op("dve", lambda e: e.tensor_scalar(out=t1[:], in0=sh["k"][:], scalar1=prm[:, 8:9], scalar2=None, op0=ALU.mult),
              reads=[sh["k"].b, prm.b], writes=[t1.b])
        cx.op("act", lambda e: e.activation(out=t2[:], in_=t1[:], func=AF.Square), reads=[t1.b], writes=[t2.b])
        p = rr("pp", pp)
        cx.op("pe", lambda e: e.matmul(p[:], lhsT=bones[:], rhs=t2[:], start=True, stop=True), reads=[bones.b, t2.b], writes=[p.b])
        cx.op("act", lambda e: e.activation(out=t2[:], in_=p[:], func=AF.Sqrt, bias=cst[:, 1:2], scale=1.0), reads=[p.b, cst.b], writes=[t2.b])
        cx.op("dve", lambda e: e.tensor_scalar(out=t2[:], in0=t2[:], scalar1=1e-12, scalar2=None, op0=ALU.max), reads=[t2.b], writes=[t2.b])
        cx.op("dve", lambda e: e.reciprocal(out=t2[:], in_=t2[:]), reads=[t2.b], writes=[t2.b])
        cx.op("dve", lambda e: e.tensor_tensor(out=kk[:], in0=t1[:], in1=t2[:], op=ALU.mult), reads=[t1.b, t2.b], writes=[kk.b])
        cx.op("dve", lambda e: e.tensor_scalar(out=t1[:], in0=arate[:], scalar1=-1.0, scalar2=prm[:, 9:10], op0=ALU.add, op1=ALU.mult),
              reads=[arate.b, prm.b], writes=[t1.b])
        cx.op("dve", lambda e: e.scalar_tensor_tensor(out=kmod[:], in0=t1[:], scalar=1.0, in1=sh["k"][:], op0=ALU.add, op1=ALU.mult),
              reads=[t1.b, sh["k"].b], writes=[kmod.b])
        cx.op("dve", lambda e: e.scalar_tensor_tensor(out=t1[:], in0=sh["r"][:], scalar=prm[:, 10:11], in1=kmod[:], op0=ALU.mult, op1=ALU.mult),
              reads=[sh["r"].b, prm.b, kmod.b], writes=[t1.b])
        p = rr("pp", pp)
        cx.op("pe", lambda e: e.matmul(p[:], lhsT=bones[:], rhs=t1[:], start=True, stop=True), reads=[bones.b, t1.b], writes=[p.b])
        cx.op("dve", lambda e: e.tensor_tensor(out=bonus[:], in0=p[:], in1=v[:], op=ALU.mult), reads=[p.b, v.b], writes=[bonus.b])
        cx.op("dve", lambda e: e.tensor_tensor_scan(out=cs[:], data0=scanm[:], data1=sig[:], initial=0.0, op0=ALU.mult, op1=ALU.add),
              reads=[scanm.b, sig.b], writes=[cs.b])
        cx.op("act", lambda e: e.activation(out=epos[:], in_=cs[:], func=AF.Exp, scale=-C0), reads=[cs.b], writes=[epos.b])
        cx.op("act", lambda e: e.activation(out=eneg[:], in_=cs[:], func=AF.Exp, scale=C0), reads=[cs.b], writes=[eneg.b])
        cx.op("dve", lambda e: e.tensor_tensor(out=t1[:], in0=cs[:], in1=sig[:], op=ALU.subtract), reads=[cs.b, sig.b], writes=[t1.b])
        cx.op("act", lambda e: e.activation(out=eprev[:], in_=t1[:], func=AF.Exp, scale=-C0), reads=[t1.b], writes=[eprev.b])
        cx.op("dve", lambda e: e.tensor_copy(out=ec[:], in_=c3(epos)[:, :, 63]), reads=[epos.b], writes=[ec.b])
        cx.op("dve", lambda e: e.tensor_tensor(out=t2[:], in0=kk[:], in1=arate[:], op=ALU.mult), reads=[kk.b, arate.b], writes=[t2.b])
        cx.op("dve", lambda e: e.tensor_tensor(out=bneg[:], in0=t2[:], in1=eneg[:], op=ALU.mult), reads=[t2.b, eneg.b], writes=[bneg.b])
        cx.op("dve", lambda e: e.tensor_tensor(out=kneg[:], in0=kmod[:], in1=eneg[:], op=ALU.mult), reads=[kmod.b, eneg.b], writes=[kneg.b])
        for (psl, csl) in HALF:
            eng = "dve"
            cx.op(eng, lambda e: e.scalar_tensor_tensor(out=bd["a"][psl, :, csl], in0=c3(kk)[psl], scalar=-1.0, in1=c3(eprev)[psl],
                                                        op0=ALU.mult, op1=ALU.mult), reads=[kk.b, eprev.b], writes=[bd["a"].b])
            cx.op(eng, lambda e: e.tensor_copy(out=bd["b"][psl, :, csl], in_=c3(bneg)[psl]), reads=[bneg.b], writes=[bd["b"].b])
            cx.op(eng, lambda e: e.tensor_copy(out=bd["k"][psl, :, csl], in_=c3(kneg)[psl]), reads=[kneg.b], writes=[bd["k"].b])
            cx.op(eng, lambda e: e.tensor_tensor(out=bd["r"][psl, :, csl], in0=c3(sh["r"])[psl], in1=c3(epos)[psl], op=ALU.mult),
                  reads=[sh["r"].b, epos.b], writes=[bd["r"].b])
            cx.op(eng, lambda e: e.tensor_copy(out=bd["v"][psl, :, csl], in_=c3(v)[psl]), reads=[v.b], writes=[bd["v"].b])
            ecb = ec[psl, :].unsqueeze(2).broadcast_to([64, 8, 64])
            cx.op(eng, lambda e: e.tensor_tensor(out=bd["bh"][psl, :, csl], in0=c3(bneg)[psl], in1=ecb, op=ALU.mult),
                  reads=[bneg.b, ec.b], writes=[bd["bh"].b])
            cx.op(eng, lambda e: e.tensor_tensor(out=bd["kh"][psl, :, csl], in0=c3(kneg)[psl], in1=ecb, op=ALU.mult),
                  reads=[kneg.b, ec.b], writes=[bd["kh"].b])
        for c in range(8):
            ct = cs_[c % 2]
            aT, bT, kT, rT, vT = (bd[q][:, c, :] for q in ["a", "b", "k", "r", "v"])
            bdr = [bd[q].b for q in ["a", "b", "k", "r", "v", "bh", "kh"]]

            def mm_mask(lhsT, rhs, mask, out_t):
                p = rr("pp", pp)
                cx.op("pe", lambda e: e.matmul(p[:, 0:128], lhsT=lhsT, rhs=rhs, start=True, stop=True), reads=bdr, writes=[p.b])
                cx.op("dve", lambda e: e.tensor_tensor(out=out_t[:], in0=p[:, 0:128], in1=mask[:], op=ALU.mult),
                      reads=[p.b, mask.b], writes=[out_t.b])

            mm_mask(bT, aT, msl, ct["LT"])
            mm_mask(aT, bT, mslT, ct["L"])
            mm_mask(kT, aT, msl, ct["AkT"])
            mm_mask(bT, rT, mil, ct["ArbT"])
            mm_mask(kT, rT, mil, ct["ArkT"])
            for i, q in enumerate(["a", "v", "bh", "kh"]):
                cx.op("pe", lambda e: e.transpose(out=ptb[:, i * 128:(i + 1) * 128], in_=bd[q][:, c, :], identity=identb[:]),
                      reads=[bd[q].b, identb.b], writes=[ptb.b], inc=(i == 3))
            tok = ct["tok"]
            cx.op("act", lambda e: e.activation(out=tok[:].rearrange("p a b -> p (a b)"), in_=ptb[:, 0:512], func=AF.Copy),
                  reads=[ptb.b], writes=[tok.b])
            Atok, Vtok, Bh, Kh = (tok[:, i, :] for i in range(4))
            X, Xb = ct["X"], ct["Xb"]
            p = rr("pp", pp)
            cx.op("pe", lambda e: e.matmul(p[:, 0:128], lhsT=ct["AkT"][:], rhs=Vtok, start=True, stop=True),
                  reads=[ct["AkT"].b, tok.b], writes=[p.b])
            cx.op("act", lambda e: e.activation(out=X[:, 128:256], in_=p[:, 0:128], func=AF.Copy), reads=[p.b], writes=[X.b])
            cx.op("dve", lambda e: e.tensor_copy(out=X[:, 0:128], in_=Atok), reads=[tok.b], writes=[X.b])
            cx.op("dve", lambda e: e.tensor_copy(out=Xb[:], in_=X[:]), reads=[X.b], writes=[Xb.b])
            Pk, PTk = ct["L"], ct["LT"]
            for lev in range(6):
                p = rr("pp", pp)
                cx.op("pe", lambda e: e.matmul(p[:, 0:256], lhsT=PTk[:], rhs=Xb[:], start=True, stop=True),
                      reads=[PTk.b, Xb.b], writes=[p.b])
                cx.op("dve", lambda e: e.tensor_tensor(out=X[:], in0=X[:], in1=p[:, 0:256], op=ALU.add), reads=[X.b, p.b], writes=[X.b])
                cx.op("act", lambda e: e.activation(out=Xb[:], in_=X[:], func=AF.Copy), reads=[X.b], writes=[Xb.b])
                if lev < 5:
                    Pn, PTn = (ct["P1"], ct["PT1"]) if lev % 2 == 0 else (ct["P2"], ct["PT2"])
                    p1 = rr("pp", pp)
                    cx.op("pe", lambda e: e.matmul(p1[:, 0:128], lhsT=PTk[:], rhs=Pk[:], start=True, stop=True),
                          reads=[PTk.b, Pk.b], writes=[p1.b])
                    cx.op("act", lambda e: e.activation(out=Pn[:], in_=p1[:, 0:128], func=AF.Copy), reads=[p1.b], writes=[Pn.b])
                    p2 = rr("pp", pp)
                    cx.op("pe", lambda e: e.matmul(p2[:, 0:128], lhsT=Pk[:], rhs=PTk[:], start=True, stop=True),
                          reads=[PTk.b, Pk.b], writes=[p2.b])
                    cx.op("dve", lambda e: e.tensor_copy(out=PTn[:], in_=p2[:, 0:128]), reads=[p2.b], writes=[PTn.b])
                    Pk, PTk = Pn, PTn
            p = rr("pp", pp)
            cx.op("pe", lambda e: e.matmul(p[:, 0:128], lhsT=Xb[:, 0:128], rhs=Bh, start=True, stop=True), reads=[Xb.b, tok.b], writes=[p.b])
            cx.op("dve", lambda e: e.scalar_tensor_tensor(out=ct["MT"][:], in0=identf[:], scalar=ec[:, c:c + 1], in1=p[:, 0:128],
                                                          op0=ALU.mult, op1=ALU.add), reads=[identf.b, ec.b, p.b], writes=[ct["MT"].b])
            p = rr("pp", pp)
            cx.op("pe", lambda e: e.matmul(p[:, 0:128], lhsT=Bh, rhs=Xb[:, 128:256], start=True, stop=False), reads=[Xb.b, tok.b], writes=[p.b], inc=False)
            cx.op("pe", lambda e: e.matmul(p[:, 0:128], lhsT=Kh, rhs=Vtok, start=False, stop=True), reads=[tok.b], writes=[p.b])
            cx.op("act", lambda e: e.activation(out=ct["Nc"][:], in_=p[:, 0:128], func=AF.Copy), reads=[p.b], writes=[ct["Nc"].b])
            p = rr("pp", pp)
            cx.op("pe", lambda e: e.matmul(p[:, 0:128], lhsT=Xb[:, 0:128], rhs=ct["ArbT"][:], start=True, stop=True),
                  reads=[Xb.b, ct["ArbT"].b], writes=[p.b])
            cx.op("dve", lambda e: e.tensor_tensor(out=ct["RbT"][:], in0=p[:, 0:128], in1=rT, op=ALU.add), reads=[p.b, bd["r"].b], writes=[ct["RbT"].b])
            p = rr("pp", pp)
            cx.op("pe", lambda e: e.matmul(p[:, 0:128], lhsT=Hb[:], rhs=ct["RbT"][:], start=True, stop=False), reads=[Hb.b, ct["RbT"].b], writes=[p.b], inc=False)
            cx.op("pe", lambda e: e.matmul(p[:, 0:128], lhsT=Xb[:, 128:256], rhs=ct["ArbT"][:], start=False, stop=False),
                  reads=[Xb.b, ct["ArbT"].b], writes=[p.b], inc=False)
            cx.op("pe", lambda e: e.matmul(p[:, 0:128], lhsT=Vtok, rhs=ct["ArkT"][:], start=False, stop=True), reads=[tok.b, ct["ArkT"].b], writes=[p.b])
            for (psl, csl) in HALF:
                cx.op("act", lambda e: e.activation(out=ysc[psl, c * 64:(c + 1) * 64], in_=p[psl, csl], func=AF.Copy), reads=[p.b], writes=[ysc.b])
            p = rr("pp", pp)
            cx.op("pe", lambda e: e.matmul(p[:, 0:128], lhsT=ct["MT"][:], rhs=Hb[:], start=True, stop=True), reads=[ct["MT"].b, Hb.b], writes=[p.b])
            cx.op("dve", lambda e: e.tensor_tensor(out=Hb[:], in0=p[:, 0:128], in1=ct["Nc"][:], op=ALU.add), reads=[p.b, ct["Nc"].b], writes=[Hb.b])
        p = rr("pp", pp)
        cx.op("pe", lambda e: e.matmul(p[:], lhsT=bones[:], rhs=ysc[:], start=True, stop=True), reads=[bones.b, ysc.b], writes=[p.b])
        cx.op("dve", lambda e: e.scalar_tensor_tensor(out=yc[:], in0=p[:], scalar=-1.0 / 64, in1=ysc[:], op0=ALU.mult, op1=ALU.add),
              reads=[p.b, ysc.b], writes=[yc.b])
        cx.op("act", lambda e: e.activation(out=t1[:], in_=yc[:], func=AF.Square), reads=[yc.b], writes=[t1.b])
        p = rr("pp", pp)
        cx.op("pe", lambda e: e.matmul(p[:], lhsT=bones[:], rhs=t1[:], start=True, stop=True), reads=[bones.b, t1.b], writes=[p.b])
        cx.op("act", lambda e: e.activation(out=t1[:], in_=p[:], func=AF.Sqrt, bias=cst[:, 0:1], scale=1.0 / 64), reads=[p.b, cst.b], writes=[t1.b])
        cx.op("dve", lambda e: e.reciprocal(out=t1[:], in_=t1[:]), reads=[t1.b], writes=[t1.b])
        cx.op("dve", lambda e: e.tensor_tensor(out=yc[:], in0=yc[:], in1=t1[:], op=ALU.mult), reads=[yc.b, t1.b], writes=[yc.b])
        cx.op("dve", lambda e: e.tensor_scalar(out=yc[:], in0=yc[:], scalar1=prm[:, 11:12], scalar2=prm[:, 12:13], op0=ALU.mult, op1=ALU.add),
              reads=[yc.b, prm.b], writes=[yc.b])
        cx.op("dve", lambda e: e.tensor_tensor(out=yc[:], in0=yc[:], in1=bonus[:], op=ALU.add), reads=[yc.b, bonus.b], writes=[yc.b])
        mo_t = mo[s % 2]
        cx.op("dve", lambda e: e.tensor_tensor(out=mo_t[:], in0=yc[:], in1=gout[:], op=ALU.mult), reads=[yc.b, gout.b], writes=[mo_t.b])
        cx.dma("sp", mix_out[128:256, tsl], mo_t[:], reads=[mo_t.b], chan=f"Rmo{s % 2}")


_PROG = {}


def _prog(key, fn):
    if key not in _PROG:
        _PROG[key] = fn()
    return _PROG[key]


def _wr(w, a, b):
    return np.ascontiguousarray(w.reshape(a, 128, b, 128).transpose(2, 1, 0, 3).reshape(b, 128, a * 128))


def _gcol(v):
    return np.ascontiguousarray(v.reshape(NDC, 128).T)


def kernel(**inputs):
    inp = {k: np.asarray(v) for k, v in inputs.items()}
    cores = list(range(NCORES))
    x = inp["x"][0]
    xT = [np.ascontiguousarray(x[c * TC:(c + 1) * TC].T).reshape(NDC, 128, TC) for c in cores]
    nc = _prog("Epre", lambda: build_E(False, True))
    g0 = _gcol(inp["norm_mix"][0])
    res = run_bass_kernel_spmd(nc, [{"xT": xT[c], "g_nxt": g0} for c in cores], core_ids=cores).results
    hs = [np.asarray(res[c]["ho"]) for c in cores]
    perm = np.concatenate([np.concatenate([np.arange(c * 128, c * 128 + 128), 1024 + np.arange(c * 128, c * 128 + 128)])
                           for c in cores])
    vfirst = None
    for l in range(DEPTH):
        hT = np.ascontiguousarray(np.concatenate(hs, axis=2))
        ncM = _prog("M", lambda: build_M(1))
        ins = []
        for c in cores:
            dd = attn_inputs(inp, l, c)
            dd.update(rwkv_inputs(inp, l, c))
            dd["hT"] = hT
            dd["vfirst"] = vfirst[c] if l > 0 else np.zeros((128, T), np.float32)
            ins.append(dd)
        res = run_bass_kernel_spmd(ncM, ins, core_ids=cores).results
        if l == 0:
            vfirst = [np.asarray(res[c]["vfirst_o"]) for c in cores]
        mixT = np.concatenate([np.asarray(res[c]["mix_out"]) for c in cores], axis=0)
        del ins
        last = (l == DEPTH - 1)
        ncE = _prog("E", lambda: build_E(True, True))
        w_out = _wr(inp["w_out"][l][perm], NDC, NDC)
        w_up = _wr(inp["mlp_up"][l], NDC, NFC)
        w_dn = _wr(inp["mlp_down"][l], NFC, NDC)
        gm = _gcol(inp["norm_mlp"][l])
        ins = []
        for c in cores:
            dd = {"xT": xT[c], "mixT": np.ascontiguousarray(mixT[:, c * TC:(c + 1) * TC]).reshape(NDC, 128, TC),
                  "w_out": w_out, "w_up": w_up, "w_dn": w_dn, "g_mlp": gm}
            dd["g_nxt"] = _gcol(inp["norm_mix"][l + 1]) if not last else np.ones((128, NDC), np.float32)
            ins.append(dd)
        res = run_bass_kernel_spmd(ncE, ins, core_ids=cores).results
        xT = [np.asarray(res[c]["xo"]) for c in cores]
        if not last:
            hs = [np.asarray(res[c]["ho"]) for c in cores]
        del ins, w_out, w_up, w_dn
    out = np.concatenate([xT[c].reshape(D, TC).T for c in cores], axis=0)[None]
    return np.ascontiguousarray(out).astype(np.float32)
```

```python
import math
import numpy as np
import ml_dtypes
import concourse.bass as bass
import concourse.mybir as mybir
from concourse.bass_utils import run_bass_kernel_spmd

F32 = mybir.dt.float32
BF16 = mybir.dt.bfloat16
ALU = mybir.AluOpType
AF = mybir.ActivationFunctionType

NCORES = 8
D = 2048
T = 16384
TC = T // NCORES
DEPTH = 4
DFF = 8192
NDC = D // 128
NFC = DFF // 128
TT = 512
RMS_EPS = 1e-6
GN_EPS = 64e-5


class Buf:
    __slots__ = ("w", "r", "excl")

    def __init__(self, excl=False):
        self.w = None
        self.r = {}
        self.excl = excl


class Ctx:
    def __init__(self, nc):
        self.nc = nc
        self.eng = {"pe": nc.tensor, "dve": nc.vector, "act": nc.scalar, "pool": nc.gpsimd, "sp": nc.sync}
        self.sems = {}
        self.val = {}
        self.waited = {e: {} for e in self.eng}
        for e in self.eng:
            self._mk(e)
        self.n_ins = 0
        self.chbufs = {}

    def _mk(self, name):
        self.sems[name] = self.nc.alloc_semaphore(name="s_" + name)
        self.val[name] = 0

    def _wait(self, e, deps):
        w = self.waited[e]
        for s, v in deps.items():
            if w.get(s, 0) < v:
                self.eng[e].wait_ge(self.sems[s], v)
                w[s] = v

    def _deps(self, e, reads, writes):
        deps = {}

        def add(tok):
            s, v = tok
            if deps.get(s, 0) < v:
                deps[s] = v

        for b in reads:
            if b.w is not None:
                if not (b.w[0] == e and e == "pe"):
                    add(b.w)
            if b.excl:
                for s, v in b.r.items():
                    if s != e:
                        add((s, v))
        for b in writes:
            if b.w is not None and not (b.w[0] == e and e == "pe"):
                add(b.w)
            for s, v in b.r.items():
                if not (s == e and e == "pe"):
                    add((s, v))
        return deps

    def _mark(self, tok, reads, writes):
        s, v = tok
        for b in reads:
            if b.r.get(s, 0) < v:
                b.r[s] = v
        for b in writes:
            b.w = tok
            b.r = {}

    def op(self, e, fn, reads=(), writes=(), inc=True):
        self._wait(e, self._deps(e, reads, writes))
        ins = fn(self.eng[e])
        self.n_ins += 1
        if inc:
            self.val[e] += 1
            ins.then_inc(self.sems[e], 1)
            tok = (e, self.val[e])
        else:
            tok = (e, self.val[e] + 1)
        self._mark(tok, reads, writes)
        return ins

    def dma(self, q, out, in_, reads=(), writes=(), chan="d0"):
        if chan not in self.sems:
            self._mk(chan)
        self._wait(q, self._deps(q, reads, writes))
        ins = self.eng[q].dma_start(out=out, in_=in_)
        self.val[chan] += 16
        ins.then_inc(self.sems[chan], 16)
        self.n_ins += 1
        self._mark((chan, self.val[chan]), reads, writes)
        self.chbufs.setdefault(chan, []).extend(writes)

    def seal(self, chan):
        for b in self.chbufs.get(chan, []):
            b.w = (chan, self.val[chan])

    def barrier(self):
        for e in self.eng:
            self._wait(e, dict(self.val))

    def finish(self):
        self._wait("sp", dict(self.val))


class TileT:
    def __init__(self, h, excl=False):
        self.h = h
        self.b = Buf(excl)

    def __getitem__(self, k):
        return self.h[k]


def sb(nc, name, shape, dt=F32):
    return TileT(nc.alloc_sbuf_tensor(name, list(shape), dt))


def ps(nc, name, shape, dt=F32):
    return TileT(nc.alloc_psum_tensor(name, list(shape), dt), True)


def build_E(do_mix, do_next):
    nc = bass.Bass("TRN2", target_bir_lowering=False)
    cx = Ctx(nc)
    NT = TC // TT
    xT = nc.dram_tensor("xT", [NDC, 128, TC], F32, kind="ExternalInput").ap()
    if do_mix:
        mixT = nc.dram_tensor("mixT", [NDC, 128, TC], BF16, kind="ExternalInput").ap()
        w_out = nc.dram_tensor("w_out", [NDC, 128, NDC * 128], F32, kind="ExternalInput").ap()
        w_up = nc.dram_tensor("w_up", [NFC, 128, NDC * 128], F32, kind="ExternalInput").ap()
        w_dn = nc.dram_tensor("w_dn", [NDC, 128, NFC * 128], F32, kind="ExternalInput").ap()
        g_mlp = nc.dram_tensor("g_mlp", [128, NDC], F32, kind="ExternalInput").ap()
        xo = nc.dram_tensor("xo", [NDC, 128, TC], F32, kind="ExternalOutput").ap()
    if do_next:
        g_nxt = nc.dram_tensor("g_nxt", [128, NDC], F32, kind="ExternalInput").ap()
        ho = nc.dram_tensor("ho", [NDC, 128, TC], BF16, kind="ExternalOutput").ap()

    x = sb(nc, "x", [128, NDC, TT])
    xb = [Buf() for _ in range(NDC)]
    h2 = sb(nc, "h2", [128, NDC, TT], BF16)
    h2b = [Buf() for _ in range(NDC)]
    sq = [sb(nc, f"sq{i}", [128, TT]) for i in range(2)]
    rt = sb(nc, "rt", [128, TT])
    rstd = sb(nc, "rstd", [128, TT])
    ones = sb(nc, "ones", [128, 128])
    epsb = sb(nc, "epsb", [128, 1])
    gm = sb(nc, "gm", [128, NDC])
    gn = sb(nc, "gn", [128, NDC])
    pacc = [ps(nc, f"pacc{i}", [128, TT]) for i in range(4)]
    pss = ps(nc, "pss", [128, TT])
    if do_mix:
        mix = sb(nc, "mix", [128, NDC, TT], BF16)
        u = sb(nc, "u", [128, NFC, TT], BF16)
        ub = [Buf() for _ in range(NFC)]
        rl = [sb(nc, f"rl{i}", [128, TT]) for i in range(2)]
        wo = [sb(nc, f"wo{i}", [128, NDC * 128], BF16) for i in range(2)]
        wu = [sb(nc, f"wu{i}", [128, NDC * 128], BF16) for i in range(3)]
        wd = [sb(nc, f"wd{i}", [128, 32 * 128], BF16) for i in range(3)]

    cx.op("dve", lambda e: e.memset(ones[:], 1.0), writes=[ones.b])
    cx.op("dve", lambda e: e.memset(epsb[:], RMS_EPS), writes=[epsb.b])
    if do_mix:
        cx.dma("sp", gm[:], g_mlp, writes=[gm.b], chan="dc")
    if do_next:
        cx.dma("sp", gn[:], g_nxt, writes=[gn.b], chan="dc")
    cx.seal("dc")

    cnt = {"acc": 0, "sq": 0, "wo": 0, "wu": 0, "wd": 0, "rl": 0}

    def rr(key, lst):
        i = cnt[key] % len(lst)
        cnt[key] += 1
        return lst[i]

    def rri(key, lst):
        i = cnt[key] % len(lst)
        cnt[key] += 1
        return lst[i], f"{key}{i}"

    def rms_to(dst, dstb, g):
        for dc in range(NDC):
            s = rr("sq", sq)
            cx.op("act", lambda e: e.activation(out=s[:], in_=x[:, dc, :], func=AF.Square),
                  reads=[xb[dc]], writes=[s.b])
            cx.op("pe", lambda e: e.matmul(pss[:], lhsT=ones[:], rhs=s[:], start=(dc == 0), stop=(dc == NDC - 1)),
                  reads=[ones.b, s.b], writes=[pss.b])
        cx.op("act", lambda e: e.activation(out=rt[:], in_=pss[:], func=AF.Sqrt, bias=epsb[:], scale=1.0 / D),
              reads=[pss.b, epsb.b], writes=[rt.b])
        cx.op("dve", lambda e: e.reciprocal(out=rstd[:], in_=rt[:]), reads=[rt.b], writes=[rstd.b])
        for dc in range(NDC):
            cx.op("dve", lambda e: e.scalar_tensor_tensor(out=dst[:, dc, :], in0=x[:, dc, :], scalar=g[:, dc:dc + 1],
                                                          in1=rstd[:], op0=ALU.mult, op1=ALU.mult),
                  reads=[xb[dc], g.b, rstd.b], writes=[dstb[dc]])

    for tt in range(NT):
        tsl = slice(tt * TT, (tt + 1) * TT)
        cx.dma("sp", x[:], xT[:, :, tsl].rearrange("c p t -> p c t"), writes=xb, chan="dx")
        if do_mix:
            cx.dma("sp", mix[:], mixT[:, :, tsl].rearrange("c p t -> p c t"), writes=[mix.b], chan="dm")
            for dc in range(NDC):
                w, ch = rri("wo", wo)
                cx.dma("pool", w[:], w_out[dc], writes=[w.b], chan=ch)
                pa = rr("acc", pacc)
                for kc in range(NDC):
                    cx.op("pe", lambda e: e.matmul(pa[:], lhsT=w[:, kc * 128:(kc + 1) * 128], rhs=mix[:, kc, :],
                                                   start=(kc == 0), stop=(kc == NDC - 1)),
                          reads=[w.b, mix.b], writes=[pa.b], inc=(kc == NDC - 1))
                cx.op("dve", lambda e: e.tensor_tensor(out=x[:, dc, :], in0=x[:, dc, :], in1=pa[:], op=ALU.add),
                      reads=[xb[dc], pa.b], writes=[xb[dc]])
            rms_to(h2, h2b, gm)
            for fc in range(NFC):
                w, ch = rri("wu", wu)
                cx.dma("pool", w[:], w_up[fc], writes=[w.b], chan=ch)
                pa = rr("acc", pacc)
                for dc in range(NDC):
                    cx.op("pe", lambda e: e.matmul(pa[:], lhsT=w[:, dc * 128:(dc + 1) * 128], rhs=h2[:, dc, :],
                                                   start=(dc == 0), stop=(dc == NDC - 1)),
                          reads=[w.b, h2b[dc]], writes=[pa.b], inc=(dc == NDC - 1))
                r = rr("rl", rl)
                cx.op("act", lambda e: e.activation(out=r[:], in_=pa[:], func=AF.Relu), reads=[pa.b], writes=[r.b])
                cx.op("dve", lambda e: e.tensor_tensor(out=u[:, fc, :], in0=r[:], in1=r[:], op=ALU.mult),
                      reads=[r.b], writes=[ub[fc]])
            for dc in range(NDC):
                pa = rr("acc", pacc)
                for hf in range(2):
                    w, ch = rri("wd", wd)
                    cx.dma("pool", w[:], w_dn[dc][:, hf * 4096:(hf + 1) * 4096], writes=[w.b], chan=ch)
                    for j in range(32):
                        fc = hf * 32 + j
                        cx.op("pe", lambda e: e.matmul(pa[:], lhsT=w[:, j * 128:(j + 1) * 128], rhs=u[:, fc, :],
                                                       start=(fc == 0), stop=(fc == NFC - 1)),
                              reads=[w.b, ub[fc]], writes=[pa.b], inc=(j == 31))
                cx.op("dve", lambda e: e.tensor_tensor(out=x[:, dc, :], in0=x[:, dc, :], in1=pa[:], op=ALU.add),
                      reads=[xb[dc], pa.b], writes=[xb[dc]])
            cx.dma("sp", xo[:, :, tsl].rearrange("c p t -> p c t"), x[:], reads=xb, chan="sx")
        if do_next:
            rms_to(h2, h2b, gn)
            cx.dma("sp", ho[:, :, tsl].rearrange("c p t -> p c t"), h2[:], reads=h2b, chan="sh")
    cx.finish()
    return nc


NTT = T // TT
NKT = T // 128
BOFF = 4
ATT_COLS = 5 * 64 + 64
SLOPES = [2.0 ** (-(i + 1)) for i in range(8)]


def alibi_tables(head):
    sl = SLOPES[head]
    kaug = np.zeros((3, T), np.float32)
    j = np.arange(T)
    kaug[0] = sl * (j % 128)
    kaug[1] = 1.0
    kaug[2] = 1.0
    qaug = np.zeros((3, TT), np.float32)
    i = np.arange(TT)
    qaug[0] = 1.0
    qaug[1] = -sl * (i % 128)
    qaug[2] = -sl * 128 * (i // 128)
    btbl = np.zeros((128, 128 + BOFF), np.float32)
    for m in range(-BOFF, 128):
        btbl[:, m + BOFF] = -sl * 128.0 * m
    jj = np.arange(128)[:, None]
    bb = np.arange(128)[None, :]
    cm = np.where(jj > bb, -2.0 * sl * (jj - bb), 0.0) + np.where((jj // 64) <= (bb // 64), 0.0, -30000.0)
    return (kaug.astype(ml_dtypes.bfloat16), qaug.astype(ml_dtypes.bfloat16), btbl, cm.astype(np.float32))


def emit_attn(nc, cx, es, l, hT, w_att, prm_a, kaug_d, qaug_d, btbl_d, cmat_d, mix_out, ntiles=NTT):
    lam_init = 0.8 - 0.6 * math.exp(-0.3 * l)

    def S(name, shape, dt=F32):
        return TileT(es.enter_context(nc.sbuf_tensor("sA_" + name, list(shape), dt)))

    def P(name, shape, dt=F32):
        return TileT(es.enter_context(nc.psum_tensor("pA_" + name, list(shape), dt)), True)

    K = [S(f"K{m}", [67, T], BF16) for m in range(2)]
    Kb = [[Buf() for _ in range(NKT)] for m in range(2)]
    V = S("V", [128, NKT, 130], BF16)
    Vb = [Buf() for _ in range(NKT)]
    W = S("Watt", [128, NDC, ATT_COLS], BF16)
    h = S("hA", [128, NDC, TT], BF16)
    qa = [[S(f"qa{m}_{i}", [67, TT], BF16) for i in range(2)] for m in range(2)]
    prm = S("prmA", [128, 8])
    gsub = S("gsub", [128, 128])
    lqk = S("lqk", [1, 4, 64])
    btbl = S("btbl", [128, 128 + BOFF])
    cmat = S("cmat", [128, 128])
    ones = S("onesA", [128, 128])
    identb = S("identb", [128, 128], BF16)
    identf = S("identf", [128, 128])
    eps = S("epsA", [128, 2])
    lam = S("lam", [128, 1])
    sqt = S("sqt", [64, 4, TT])
    rtt = S("rtt", [64, 4, TT])
    rst = S("rst", [64, 4, TT])
    pt = [S(f"pt{i}", [128, TT], BF16) for i in range(4)]
    dg = [S(f"dg{i}", [128, 128]) for i in range(2)]
    fin = {k: S("fin_" + k, s, d) for k, s, d in [("r1", [128, 1], F32), ("r2", [128, 1], F32), ("t2", [128, 128], F32),
                                                  ("t", [128, 128], F32), ("sq", [128, 128], F32), ("ss", [128, 1], F32),
                                                  ("rt", [128, 1], F32), ("rs", [128, 1], F32), ("an", [128, 128], BF16)]}
    mo = [S(f"mo{i}", [128, TT], BF16) for i in range(2)]
    tiny = S("tiny", [1, 8])
    pq = [P(f"pq{i}", [128, TT]) for i in range(3)]
    po = [[P(f"po{m}_{i}", [128, 2, 256]) for i in range(2)] for m in range(2)]
    pT = P("pT", [128, 1024], BF16)
    pob = [[[po[m][i].b, po[m][i].b] for i in range(2)] for m in range(2)]

    cx.op("dve", lambda e: e.memset(ones[:], 1.0), writes=[ones.b])
    cx.op("dve", lambda e: e.memset(eps[:, 0:1], 64.0 * RMS_EPS), writes=[eps.b])
    cx.op("dve", lambda e: e.memset(eps[:, 1:2], RMS_EPS), writes=[eps.b])
    cx.op("pool", lambda e: e.memset(identf[:], 1.0), writes=[identf.b])
    cx.op("pool", lambda e: e.affine_select(out=identf[:], in_=identf[:], pattern=[[-1, 128]], compare_op=ALU.is_equal,
                                            fill=0.0, base=0, channel_multiplier=1), reads=[identf.b], writes=[identf.b])
    cx.op("dve", lambda e: e.tensor_copy(out=identb[:], in_=identf[:]), reads=[identf.b], writes=[identb.b])
    cx.op("dve", lambda e: e.memset(V[:], 1.0), writes=Vb)
    cx.dma("sp", prm[:], prm_a["prm"], writes=[prm.b], chan="dc")
    cx.dma("sp", gsub[:], prm_a["gsub"], writes=[gsub.b], chan="dc")
    cx.dma("sp", lqk[:], prm_a["lqk"], writes=[lqk.b], chan="dc")
    cx.dma("sp", btbl[:], btbl_d, writes=[btbl.b], chan="dc")
    cx.dma("sp", cmat[:], cmat_d, writes=[cmat.b], chan="dc")
    for m in range(2):
        cx.dma("sp", K[m][64:67, :], kaug_d, writes=Kb[m], chan="dc")
        for i in range(2):
            cx.dma("sp", qa[m][i][64:67, :], qaug_d, writes=[qa[m][i].b], chan="dc")
    cx.seal("dc")
    cx.dma("pool", W[:], w_att, writes=[W.b], chan="dW")
    cx.op("dve", lambda e: e.tensor_tensor(out=lqk[:, 0:2, :], in0=lqk[:, 0:2, :], in1=lqk[:, 2:4, :], op=ALU.mult),
          reads=[lqk.b], writes=[lqk.b])
    cx.op("dve", lambda e: e.tensor_reduce(out=tiny[:, 0:2], in_=lqk[:, 0:2, :], axis=mybir.AxisListType.X, op=ALU.add),
          reads=[lqk.b], writes=[tiny.b])
    cx.op("act", lambda e: e.activation(out=tiny[:, 2:4], in_=tiny[:, 0:2], func=AF.Exp), reads=[tiny.b], writes=[tiny.b])
    cx.op("dve", lambda e: e.tensor_tensor(out=tiny[:, 4:5], in0=tiny[:, 2:3], in1=tiny[:, 3:4], op=ALU.subtract),
          reads=[tiny.b], writes=[tiny.b])
    cx.op("dve", lambda e: e.tensor_scalar(out=tiny[:, 5:6], in0=tiny[:, 4:5], scalar1=prm[0:1, 4:5], scalar2=None, op0=ALU.add),
          reads=[tiny.b, prm.b], writes=[tiny.b])
    cx.op("pe", lambda e: e.matmul(pq[0][:, 0:1], lhsT=ones[0:1, :], rhs=tiny[:, 5:6], start=True, stop=True),
          reads=[ones.b, tiny.b], writes=[pq[0].b])
    cx.op("dve", lambda e: e.tensor_copy(out=lam[:], in_=pq[0][:, 0:1]), reads=[pq[0].b], writes=[lam.b])
    cx.op("dve", lambda e: e.tensor_scalar(out=gsub[:], in0=gsub[:], scalar1=prm[:, 5:6], scalar2=None, op0=ALU.mult),
          reads=[gsub.b, prm.b], writes=[gsub.b])

    cnt = {"pq": 0, "pt": 0, "dg": 0}

    def rr(key, lst):
        i = cnt[key] % len(lst)
        cnt[key] += 1
        return lst[i]

    import os
    STOP = int(os.environ.get("DBG_STOP", "99"))
    SUB = int(os.environ.get("DBG_SUB", "99"))
    if STOP < 1:
        return
    for s in range(ntiles):
        tsl = slice(s * TT, (s + 1) * TT)
        cx.dma("sp", h[:], hT[:, :, tsl].rearrange("c p t -> p c t"), writes=[h.b], chan="dh")
        q = [qa[0][s % 2], qa[1][s % 2]]
        pg = [rr("pq", pq) for _ in range(4)]
        for g in range(4):
            for dc in range(NDC):
                cx.op("pe", lambda e: e.matmul(pg[g][0:64, :], lhsT=W[:, dc, g * 64:(g + 1) * 64], rhs=h[:, dc, :],
                                               start=(dc == 0), stop=(dc == NDC - 1)),
                      reads=[W.b, h.b], writes=[pg[g].b], inc=(dc == NDC - 1))
            if SUB < 2:
                continue
            cx.op("act", lambda e: e.activation(out=sqt[:, g, :], in_=pg[g][0:64, :], func=AF.Square),
                  reads=[pg[g].b], writes=[sqt.b])
            cx.op("dve", lambda e: e.tensor_copy(out=rtt[:, g, :], in_=pg[g][0:64, :]), reads=[pg[g].b], writes=[rtt.b])
        if SUB < 3:
            continue
        for g in range(4):
            cx.op("pe", lambda e: e.matmul(pg[g][0:64, :], lhsT=ones[0:64, 0:64], rhs=sqt[:, g, :], start=True, stop=True),
                  reads=[ones.b, sqt.b, rtt.b], writes=[pg[g].b])
            if SUB < 4:
                continue
            if g < 2:
                cx.op("act", lambda e: e.activation(out=sqt[:, g, :], in_=pg[g][0:64, :], func=AF.Sqrt, bias=eps[0:64, 0:1],
                                                    scale=1.0), reads=[pg[g].b, eps.b], writes=[sqt.b])
            else:
                cx.op("act", lambda e: e.activation(out=sqt[:, g, :], in_=pg[g][0:64, :], func=AF.Sqrt, bias=eps[0:64, 1:2],
                                                    scale=1.0 / 64), reads=[pg[g].b, eps.b], writes=[sqt.b])
        if SUB < 5:
            continue
        cx.op("dve", lambda e: e.reciprocal(out=rst[:], in_=sqt[:]), reads=[sqt.b], writes=[rst.b])
        if SUB < 6:
            continue
        for g in range(4):
            if g < 2:
                dst, dstb = q[g][0:64, :], q[g].b
            else:
                dst, dstb = K[g - 2][0:64, tsl], None
            wr = [dstb] if dstb is not None else Kb[g - 2][4 * s:4 * s + 4]
            cx.op("dve", lambda e: e.scalar_tensor_tensor(out=dst, in0=rtt[:, g, :], scalar=prm[0:64, g:g + 1], in1=rst[:, g, :],
                                                          op0=ALU.mult, op1=ALU.mult),
                  reads=[rtt.b, prm.b, rst.b], writes=wr)
        if STOP < 2:
            continue
        for sub in range(4):
            pv = rr("pq", pq)
            for dc in range(NDC):
                cx.op("pe", lambda e: e.matmul(pv[:, 0:128], lhsT=h[:, dc, sub * 128:(sub + 1) * 128], rhs=W[:, dc, 256:384],
                                               start=(dc == 0), stop=(dc == NDC - 1)),
                      reads=[W.b, h.b], writes=[pv.b], inc=(dc == NDC - 1))
            cx.op("act", lambda e: e.activation(out=V[:, 4 * s + sub, 0:128], in_=pv[:, 0:128], func=AF.Copy),
                  reads=[pv.b], writes=[Vb[4 * s + sub]])
        if STOP < 3:
            continue
        nk = 4 * s + 4
        units = [(kt, m) for kt in range(nk) for m in range(2)]

        def qk(kt, m):
            pS = rr("pq", pq)
            p = rr("pt", pt)
            mm = kt - 4 * s
            c0 = 128 * mm if mm > 0 else 0
            cx.op("pe", lambda e: e.matmul(pS[:, c0:TT], lhsT=K[m][0:67, kt * 128:(kt + 1) * 128], rhs=q[m][0:67, c0:TT],
                                           start=True, stop=True), reads=[Kb[m][kt], q[m].b], writes=[pS.b])
            bcol = BOFF + (4 * s - kt)
            if mm >= 0:
                d = rr("dg", dg)
                cx.op("dve", lambda e: e.tensor_tensor(out=d[:], in0=pS[:, c0:c0 + 128], in1=cmat[:], op=ALU.add),
                      reads=[pS.b, cmat.b], writes=[d.b])
                cx.op("act", lambda e: e.activation(out=p[:, c0:c0 + 128], in_=d[:], func=AF.Exp, bias=btbl[:, bcol:bcol + 1],
                                                    scale=1.0), reads=[d.b, btbl.b], writes=[p.b])
                if c0 + 128 < TT:
                    cx.op("act", lambda e: e.activation(out=p[:, c0 + 128:TT], in_=pS[:, c0 + 128:TT], func=AF.Exp,
                                                        bias=btbl[:, bcol:bcol + 1], scale=1.0),
                          reads=[pS.b, btbl.b], writes=[p.b])
            else:
                cx.op("act", lambda e: e.activation(out=p[:], in_=pS[:], func=AF.Exp, bias=btbl[:, bcol:bcol + 1], scale=1.0),
                      reads=[pS.b, btbl.b], writes=[p.b])
            return p

        def pvmm(kt, m, p):
            mm = kt - 4 * s
            a0 = mm if mm > 0 else 0
            for a in range(a0, 4):
                cx.op("pe", lambda e: e.matmul(po[m][a // 2][:, a % 2, 0:129], lhsT=p[:, a * 128:(a + 1) * 128],
                                               rhs=V[:, kt, 0:129], start=(kt == 0 and a % 2 == 0), stop=(kt == 4 * s + a),
                                               skip_group_check=True),
                      reads=[p.b, Vb[kt]], writes=[pob[m][a // 2][a % 2]], inc=(a == 3))

        LOOK = 2
        pend = []
        for (kt, m) in units:
            p = qk(kt, m)
            pend.append((kt, m, p))
            if len(pend) > LOOK:
                pvmm(*pend.pop(0))
        while pend:
            pvmm(*pend.pop(0))
        if STOP < 4:
            continue
        mo_t = mo[s % 2]
        for a in range(4):
            o1 = po[0][a // 2]
            o2 = po[1][a // 2]
            b1 = pob[0][a // 2][a % 2]
            b2 = pob[1][a // 2][a % 2]
            f = fin
            cx.op("dve", lambda e: e.reciprocal(out=f["r1"][:], in_=o1[:, a % 2, 128:129]), reads=[b1], writes=[f["r1"].b])
            cx.op("dve", lambda e: e.reciprocal(out=f["r2"][:], in_=o2[:, a % 2, 128:129]), reads=[b2], writes=[f["r2"].b])
            cx.op("dve", lambda e: e.tensor_tensor(out=f["r2"][:], in0=f["r2"][:], in1=lam[:], op=ALU.mult),
                  reads=[f["r2"].b, lam.b], writes=[f["r2"].b])
            cx.op("dve", lambda e: e.tensor_scalar(out=f["t2"][:], in0=o2[:, a % 2, 0:128], scalar1=f["r2"][:, 0:1], scalar2=None,
                                                   op0=ALU.mult), reads=[b2, f["r2"].b], writes=[f["t2"].b])
            cx.op("dve", lambda e: e.scalar_tensor_tensor(out=f["t"][:], in0=o1[:, a % 2, 0:128], scalar=f["r1"][:, 0:1],
                                                          in1=f["t2"][:], op0=ALU.mult, op1=ALU.subtract),
                  reads=[b1, f["r1"].b, f["t2"].b], writes=[f["t"].b])
            cx.op("act", lambda e: e.activation(out=f["sq"][:], in_=f["t"][:], func=AF.Square, accum_out=f["ss"][:]),
                  reads=[f["t"].b], writes=[f["sq"].b, f["ss"].b])
            cx.op("act", lambda e: e.activation(out=f["rt"][:], in_=f["ss"][:], func=AF.Sqrt, bias=eps[:, 1:2], scale=1.0 / 128),
                  reads=[f["ss"].b, eps.b], writes=[f["rt"].b])
            cx.op("dve", lambda e: e.reciprocal(out=f["rs"][:], in_=f["rt"][:]), reads=[f["rt"].b], writes=[f["rs"].b])
            cx.op("dve", lambda e: e.scalar_tensor_tensor(out=f["an"][:], in0=f["t"][:], scalar=f["rs"][:, 0:1], in1=gsub[:],
                                                          op0=ALU.mult, op1=ALU.mult),
                  reads=[f["t"].b, f["rs"].b, gsub.b], writes=[f["an"].b])
            cx.op("pe", lambda e: e.transpose(out=pT[:, 0:128], in_=f["an"][:], identity=identb[:]),
                  reads=[f["an"].b, identb.b], writes=[pT.b])
            cx.op("act", lambda e: e.activation(out=mo_t[:, a * 128:(a + 1) * 128], in_=pT[:, 0:128], func=AF.Copy),
                  reads=[pT.b], writes=[mo_t.b])
        cx.dma("sp", mix_out[0:128, tsl], mo_t[:], reads=[mo_t.b], chan=f"mo{s % 2}")


def build_M(l, do_attn=True, do_rwkv=True, ntiles=NTT):
    from contextlib import ExitStack
    nc = bass.Bass("TRN2", target_bir_lowering=False)
    cx = Ctx(nc)
    NTK = ntiles * TT
    hT = nc.dram_tensor("hT", [NDC, 128, NTK], BF16, kind="ExternalInput").ap()
    mix_out = nc.dram_tensor("mix_out", [256, NTK], BF16, kind="ExternalOutput").ap()

    def din(name, shape, dt=F32):
        return nc.dram_tensor(name, list(shape), dt, kind="ExternalInput").ap()

    if do_attn:
        w_att = din("w_att", [128, NDC, ATT_COLS])
        prm_a = {"prm": din("prm_a", [128, 8]), "gsub": din("gsub", [128, 128]), "lqk": din("lqk", [1, 4, 64])}
        kaug = din("kaug", [3, T], BF16)
        qaug = din("qaug", [3, TT], BF16)
        btbl = din("btbl", [128, 128 + BOFF])
        cmat = din("cmat", [128, 128])
        with ExitStack() as es:
            emit_attn(nc, cx, es, l, hT, w_att, prm_a, kaug, qaug, btbl, cmat, mix_out, ntiles)
            cx.barrier()
    if do_rwkv:
        l = 1
        nch = 14
        d = {"w_r": din("w_r", [nch, 128, NDC, 128]), "prm_r": din("prm_r", [128, NPR]), "w2a2": din("w2a2", [128, 128]),
             "g2": din("g2", [160, 128]), "msl": din("msl", [128, 128]), "mslT": din("mslT", [128, 128]),
             "mil": din("mil", [128, 128]), "bones": din("bones", [128, 128]), "scanm": din("scanm", [128, TT])}
        d["v1"] = din("v1", [128, 8, 32])
        d["v2"] = din("v2", [32, 128])
        d["vfirst"] = din("vfirst", [128, NTK])
        d["vfirst_o"] = nc.dram_tensor("vfirst_o", [128, NTK], F32, kind="ExternalOutput").ap()
        with ExitStack() as es:
            emit_rwkv(nc, cx, es, l, hT, d, mix_out, ntiles)
            cx.barrier()
    cx.finish()
    return nc


def rwkv_inputs(inp, l, c):
    w = inp["w_in"][l]
    own = np.arange(c * 128, c * 128 + 128)
    base = 3072
    chunks = [base + own, base + 1024 + own, base + 2048 + own, base + 3072 + np.arange(128)]
    chunks.append(base + 3072 + 128 + np.arange(128))
    g1 = np.full(128, -1)
    g1[0:32] = base + 3072 + 256 + np.arange(32)
    chunks.append(g1)
    for j in range(8):
        chunks.append(base + 2048 + j * 128 + np.arange(128))
    w_r = np.zeros((len(chunks), 128, NDC, 128), np.float32)
    for i, cols in enumerate(chunks):
        valid = cols >= 0
        blk = np.zeros((D, 128), np.float32)
        blk[:, valid] = w[:, cols[valid]]
        w_r[i] = blk.reshape(NDC, 128, 128).transpose(1, 0, 2)
    mu = inp["rwkv_mu"][l]
    prm = np.zeros((128, NPR), np.float32)
    prm[:, 0] = mu[own]
    prm[:, 1] = mu[1024 + own]
    prm[:, 2] = mu[2048 + own]
    prm[:, 3] = mu[3072:3072 + 128]
    prm[:, 4] = mu[3072 + 128:3072 + 256]
    prm[0:32, 5] = mu[3072 + 256:3072 + 288]
    prm[:, 6] = inp["rwkv_w0"][l][own]
    prm[:, 7] = inp["rwkv_a0"][l][own]
    prm[:, 8] = inp["rwkv_k_k"][l][own]
    prm[:, 9] = inp["rwkv_k_a"][l][own]
    prm[:, 10] = inp["rwkv_r_k"][l].reshape(-1)[own]
    prm[:, 11] = inp["rwkv_ln_w"][l][own]
    prm[:, 12] = inp["rwkv_ln_b"][l][own]
    out = {"w_r": w_r, "w2a2": np.concatenate([inp["rwkv_w2"][l][:, own], inp["rwkv_a2"][l][:, own]], 0).astype(np.float32),
           "g2": np.ascontiguousarray(inp["rwkv_g2"][l][:, own])}
    for j in range(8):
        prm[:, 14 + j] = mu[2048 + j * 128:2048 + (j + 1) * 128]
    if l > 0:
        prm[:, 13] = inp["rwkv_v0"][l - 1][own]
        out["v1"] = np.ascontiguousarray(inp["rwkv_v1"][l - 1].reshape(8, 128, 32).transpose(1, 0, 2))
        out["v2"] = np.ascontiguousarray(inp["rwkv_v2"][l - 1][:, own])
    else:
        prm[:, 13] = -30000.0
        out["v1"] = np.zeros((128, 8, 32), np.float32)
        out["v2"] = np.zeros((32, 128), np.float32)
    out["prm_r"] = prm
    out.update(rwkv_consts())
    return out


def attn_inputs(inp, l, c):
    w = inp["w_in"][l]
    cols = np.concatenate([np.arange(c * 128, c * 128 + 128), 1024 + np.arange(c * 128, c * 128 + 128),
                           2048 + np.arange(c * 128, c * 128 + 128)])
    w_att = np.ascontiguousarray(w[:, cols].reshape(NDC, 128, ATT_COLS).transpose(1, 0, 2))
    prm = np.zeros((128, 8), np.float32)
    prm[0:64, 0] = inp["qk_norm_q"][l][0]
    prm[0:64, 1] = inp["qk_norm_q"][l][1]
    prm[0:64, 2] = inp["qk_norm_k"][l][0]
    prm[0:64, 3] = inp["qk_norm_k"][l][1]
    lam_init = 0.8 - 0.6 * math.exp(-0.3 * l)
    prm[:, 4] = lam_init
    prm[:, 5] = 1.0 - lam_init
    gsub = np.ascontiguousarray(np.broadcast_to(inp["diff_subln"][l][None, :], (128, 128))).astype(np.float32)
    lqk = np.stack([inp["diff_lambda_q"][l][0], inp["diff_lambda_q"][l][1], inp["diff_lambda_k"][l][0],
                    inp["diff_lambda_k"][l][1]])[None].astype(np.float32)
    kaug, qaug, btbl, cmat = alibi_tables(c)
    return {"w_att": w_att, "prm_a": prm, "gsub": gsub, "lqk": lqk, "kaug": kaug, "qaug": qaug, "btbl": btbl, "cmat": cmat}


C0 = math.exp(-0.5)
NPR = 24


def rwkv_consts():
    hh = np.arange(128) // 64
    tt = np.arange(128) % 64
    same = hh[:, None] == hh[None, :]
    msl = (same & (tt[:, None] < tt[None, :])).astype(np.float32)
    mslT = np.ascontiguousarray(msl.T)
    mil = (same & (tt[:, None] <= tt[None, :])).astype(np.float32)
    bones = same.astype(np.float32)
    scanm = np.ones((128, TT), np.float32)
    scanm[:, ::64] = 0.0
    return {"msl": msl, "mslT": mslT, "mil": mil, "bones": bones, "scanm": scanm}


def emit_rwkv(nc, cx, es, l, hT, d, mix_out, ntiles=NTT):
    nch = 6 + (8 if l > 0 else 0)

    def S(name, shape, dt=F32):
        return TileT(es.enter_context(nc.sbuf_tensor("sR_" + name, list(shape), dt)))

    def P(name, shape, dt=F32):
        return TileT(es.enter_context(nc.psum_tensor("pR_" + name, list(shape), dt)), True)

    h = S("h", [128, NDC, TT], BF16)
    wr = [S(f"wr{i}", [128, NDC * 128], BF16) for i in range(2)]
    prm = S("prm", [128, NPR])
    w2a2 = S("w2a2", [128, 128], BF16)
    g2t = S("g2t", [128, 2, 128], BF16)
    msl = S("msl", [128, 128]); mslT = S("mslT", [128, 128]); mil = S("mil", [128, 128])
    bones = S("bones", [128, 128]); scanm = S("scanm", [128, TT])
    identf = S("identf", [128, 128]); identb = S("identb", [128, 128], BF16)
    cst = S("cst", [128, 4])
    raw = {q: S("raw_" + q, [128, TT + 1]) for q in ["r", "k", "v", "wa", "g0", "g1"]}
    sh = {q: S("sh_" + q, [128, TT]) for q in ["r", "k", "v", "wa", "g0", "g1"]}
    tmp = [S(f"tmp{i}", [128, TT]) for i in range(3)]
    wab = S("wab", [128, TT], BF16)
    sg0 = S("sg0", [128, TT], BF16); sg1 = S("sg1", [32, TT], BF16)
    sig = S("sig", [128, TT]); arate = S("arate", [128, TT]); gout = S("gout", [128, TT])
    kk = S("kk", [128, TT]); kmod = S("kmod", [128, TT]); bonus = S("bonus", [128, TT])
    cs = S("cs", [128, TT]); epos = S("epos", [128, TT]); eneg = S("eneg", [128, TT]); eprev = S("eprev", [128, TT])
    ec = S("ec", [128, 8]); bneg = S("bneg", [128, TT]); kneg = S("kneg", [128, TT])
    ysc = S("ysc", [128, TT]); yc = S("yc", [128, TT])
    bd = {q: S("bd_" + q, [128, 8, 128], BF16) for q in ["a", "b", "k", "r", "v", "bh", "kh"]}
    mo = [S(f"mo{i}", [128, TT], BF16) for i in range(2)]
    Hb = S("Hb", [128, 128], BF16)
    def cset(i):
        t = {}
        for n in ["LT", "L", "AkT", "ArbT", "ArkT", "MT", "RbT", "P1", "PT1", "P2", "PT2"]:
            t[n] = S(f"c{i}_{n}", [128, 128], BF16)
        t["tok"] = S(f"c{i}_tok", [128, 4, 128], BF16)
        t["X"] = S(f"c{i}_X", [128, 256])
        t["Xb"] = S(f"c{i}_Xb", [128, 256], BF16)
        t["Nc"] = S(f"c{i}_Nc", [128, 128])
        return t
    cs_ = [cset(0), cset(1)]
    if l > 0:
        v1t = S("v1t", [128, 8, 32], BF16); v2t = S("v2t", [32, 128], BF16)
        cvf = S("cvf", [128, 8]); rawx = S("rawx", [128, TT + 1])
        vfsh = S("vfsh", [128, 8, TT], BF16); lvb = S("lvb", [32, TT], BF16)
        vft = S("vft", [128, TT]); gate = S("gate", [128, TT])
    pp = [P(f"p{i}", [128, TT]) for i in range(7)]
    ptb = P("ptb", [128, 1024], BF16)

    cnt = {"pp": 0, "wr": 0}

    def rr(key, lst):
        i = cnt[key] % len(lst)
        cnt[key] += 1
        return lst[i]

    def rri(key, lst):
        i = cnt[key] % len(lst)
        cnt[key] += 1
        return lst[i], f"R{key}{i}"

    for name, t in [("msl", msl), ("mslT", mslT), ("mil", mil), ("bones", bones), ("scanm", scanm), ("prm_r", prm)]:
        cx.dma("sp", t[:], d[name], writes=[t.b], chan="dcR")
    cx.seal("dcR")
    cx.dma("pool", w2a2[:], d["w2a2"], writes=[w2a2.b], chan="dcR2")
    cx.dma("pool", g2t[:, 0, :], d["g2"][0:128, :], writes=[g2t.b], chan="dcR2")
    cx.dma("pool", g2t[0:32, 1, :], d["g2"][128:160, :], writes=[g2t.b], chan="dcR2")
    if l > 0:
        cx.dma("pool", v1t[:], d["v1"], writes=[v1t.b], chan="dcR2")
        cx.dma("pool", v2t[:], d["v2"], writes=[v2t.b], chan="dcR2")
        cx.op("dve", lambda e: e.memset(cvf[:], 0.0), writes=[cvf.b])
    cx.seal("dcR2")
    cx.op("pool", lambda e: e.memset(identf[:], 1.0), writes=[identf.b])
    cx.op("pool", lambda e: e.affine_select(out=identf[:], in_=identf[:], pattern=[[-1, 128]], compare_op=ALU.is_equal,
                                            fill=0.0, base=0, channel_multiplier=1), reads=[identf.b], writes=[identf.b])
    cx.op("dve", lambda e: e.tensor_copy(out=identb[:], in_=identf[:]), reads=[identf.b], writes=[identb.b])
    cx.op("dve", lambda e: e.memset(cst[:, 0:1], GN_EPS), writes=[cst.b])
    cx.op("dve", lambda e: e.memset(cst[:, 1:2], 0.0), writes=[cst.b])
    cx.op("dve", lambda e: e.memset(Hb[:], 0.0), writes=[Hb.b])
    for q in bd:
        cx.op("pool", lambda e: e.memset(bd[q][:], 0.0), writes=[bd[q].b])
    for q in raw:
        cx.op("dve", lambda e: e.memset(raw[q][:], 0.0), writes=[raw[q].b])

    def c3(t):
        return t.h[:, :].rearrange("p (c t) -> p c t", t=64)

    HALF = [(slice(0, 64), slice(0, 64)), (slice(64, 128), slice(64, 128))]

    for s in range(ntiles):
        tsl = slice(s * TT, (s + 1) * TT)
        cx.dma("sp", h[:], hT[:, :, tsl].rearrange("c p t -> p c t"), writes=[h.b], chan="dhR")

        def inproj(ch, M):
            w, chn = rri("wr", wr)
            cx.dma("pool", w[:], d["w_r"][ch].rearrange("p c j -> p (c j)"), writes=[w.b], chan=chn)
            p = rr("pp", pp)
            for dc in range(NDC):
                cx.op("pe", lambda e: e.matmul(p[0:M, :], lhsT=w[:, dc * 128:dc * 128 + M], rhs=h[:, dc, :],
                                               start=(dc == 0), stop=(dc == NDC - 1)),
                      reads=[w.b, h.b], writes=[p.b], inc=(dc == NDC - 1))
            return p

        def shift(p, rw, out_ap, outb, mu_ap, M=128):
            cx.op("act", lambda e: e.activation(out=rw[0:M, 1:TT + 1], in_=p[0:M, :], func=AF.Copy), reads=[p.b], writes=[rw.b])
            t0 = tmp[0]
            cx.op("dve", lambda e: e.tensor_tensor(out=t0[0:M, :], in0=rw[0:M, 0:TT], in1=rw[0:M, 1:TT + 1], op=ALU.subtract),
                  reads=[rw.b], writes=[t0.b])
            cx.op("dve", lambda e: e.scalar_tensor_tensor(out=out_ap, in0=t0[0:M, :], scalar=mu_ap, in1=rw[0:M, 1:TT + 1],
                                                          op0=ALU.mult, op1=ALU.add), reads=[t0.b, rw.b, prm.b], writes=[outb])

        for qi, q in enumerate(["r", "k", "v", "wa", "g0", "g1"]):
            M = 32 if q == "g1" else 128
            p = inproj(qi, M)
            rw = raw[q]
            shift(p, rw, sh[q][0:M, :], sh[q].b, prm[0:M, qi:qi + 1], M)
            cx.op("dve", lambda e: e.tensor_copy(out=rw[0:M, 0:1], in_=rw[0:M, TT:TT + 1]), reads=[rw.b], writes=[rw.b])
        v = sh["v"]
        if l > 0:
            for j in range(8):
                p = inproj(6 + j, 128)
                cx.op("dve", lambda e: e.tensor_copy(out=rawx[:, 0:1], in_=cvf[:, j:j + 1]), reads=[cvf.b], writes=[rawx.b])
                shift(p, rawx, vfsh[:, j, :], vfsh.b, prm[:, 14 + j:15 + j])
                cx.op("dve", lambda e: e.tensor_copy(out=cvf[:, j:j + 1], in_=rawx[:, TT:TT + 1]), reads=[rawx.b], writes=[cvf.b])
            p = rr("pp", pp)
            for j in range(8):
                cx.op("pe", lambda e: e.matmul(p[0:32, :], lhsT=v1t[:, j, :], rhs=vfsh[:, j, :], start=(j == 0), stop=(j == 7)),
                      reads=[v1t.b, vfsh.b], writes=[p.b], inc=(j == 7))
            cx.op("act", lambda e: e.activation(out=lvb[:], in_=p[0:32, :], func=AF.Copy), reads=[p.b], writes=[lvb.b])
            p = rr("pp", pp)
            cx.op("pe", lambda e: e.matmul(p[:], lhsT=v2t[:], rhs=lvb[:], start=True, stop=True), reads=[v2t.b, lvb.b], writes=[p.b])
            cx.op("act", lambda e: e.activation(out=gate[:], in_=p[:], func=AF.Sigmoid, bias=prm[:, 13:14], scale=1.0),
                  reads=[p.b, prm.b], writes=[gate.b])
            cx.dma("sp", vft[:], d["vfirst"][:, tsl], writes=[vft.b], chan="dvf")
            cx.op("dve", lambda e: e.tensor_tensor(out=vft[:], in0=vft[:], in1=v[:], op=ALU.subtract), reads=[vft.b, v.b], writes=[vft.b])
            cx.op("dve", lambda e: e.tensor_tensor(out=vft[:], in0=vft[:], in1=gate[:], op=ALU.mult), reads=[vft.b, gate.b], writes=[vft.b])
            cx.op("dve", lambda e: e.tensor_tensor(out=v[:], in0=v[:], in1=vft[:], op=ALU.add), reads=[vft.b, v.b], writes=[v.b])
        cx.dma("sp", d["vfirst_o"][:, tsl], v[:], reads=[v.b], chan="svf")
        cx.op("act", lambda e: e.activation(out=wab[0:64, :], in_=sh["wa"][0:64, :], func=AF.Tanh), reads=[sh["wa"].b], writes=[wab.b])
        cx.op("dve", lambda e: e.tensor_copy(out=wab[64:128, :], in_=sh["wa"][64:128, :]), reads=[sh["wa"].b], writes=[wab.b])
        p = rr("pp", pp)
        cx.op("pe", lambda e: e.matmul(p[:], lhsT=w2a2[0:64, :], rhs=wab[0:64, :], start=True, stop=True), reads=[w2a2.b, wab.b], writes=[p.b])
        cx.op("act", lambda e: e.activation(out=sig[:], in_=p[:], func=AF.Sigmoid, bias=prm[:, 6:7], scale=1.0),
              reads=[p.b, prm.b], writes=[sig.b])
        p = rr("pp", pp)
        cx.op("pe", lambda e: e.matmul(p[:], lhsT=w2a2[64:128, :], rhs=wab[64:128, :], start=True, stop=True), reads=[w2a2.b, wab.b], writes=[p.b])
        cx.op("act", lambda e: e.activation(out=arate[:], in_=p[:], func=AF.Sigmoid, bias=prm[:, 7:8], scale=1.0),
              reads=[p.b, prm.b], writes=[arate.b])
        cx.op("act", lambda e: e.activation(out=sg0[:], in_=sh["g0"][:], func=AF.Sigmoid), reads=[sh["g0"].b], writes=[sg0.b])
        cx.op("act", lambda e: e.activation(out=sg1[:], in_=sh["g1"][0:32, :], func=AF.Sigmoid), reads=[sh["g1"].b], writes=[sg1.b])
        p = rr("pp", pp)
        cx.op("pe", lambda e: e.matmul(p[:], lhsT=g2t[:, 0, :], rhs=sg0[:], start=True, stop=False), reads=[g2t.b, sg0.b], writes=[p.b], inc=False)
        cx.op("pe", lambda e: e.matmul(p[:], lhsT=g2t[0:32, 1, :], rhs=sg1[:], start=False, stop=True), reads=[g2t.b, sg1.b], writes=[p.b])
        cx.op("act", lambda e: e.activation(out=gout[:], in_=p[:], func=AF.Copy), reads=[p.b], writes=[gout.b])
        t1, t2 = tmp[1], tmp[2]
        cx.op("dve", lambda e: e.tensor_scalar(out=t1[:], in0=sh["k"][:], scalar1=prm[:, 8:9], scalar2=None, op0=ALU.mult),
              reads=[sh["k"].b, prm.b], writes=[t1.b])
        cx.op("act", lambda e: e.activation(out=t2[:], in_=t1[:], func=AF.Square), reads=[t1.b], writes=[t2.b])
        p = rr("pp", pp)
        cx.op("pe", lambda e: e.matmul(p[:], lhsT=bones[:], rhs=t2[:], start=True, stop=True), reads=[bones.b, t2.b], writes=[p.b])
        cx.op("act", lambda e: e.activation(out=t2[:], in_=p[:], func=AF.Sqrt, bias=cst[:, 1:2], scale=1.0), reads=[p.b, cst.b], writes=[t2.b])
        cx.op("dve", lambda e: e.tensor_scalar(out=t2[:], in0=t2[:], scalar1=1e-12, scalar2=None, op0=ALU.max), reads=[t2.b], writes=[t2.b])
        cx.op("dve", lambda e: e.reciprocal(out=t2[:], in_=t2[:]), reads=[t2.b], writes=[t2.b])
        cx.op("dve", lambda e: e.tensor_tensor(out=kk[:], in0=t1[:], in1=t2[:], op=ALU.mult), reads=[t1.b, t2.b], writes=[kk.b])
        cx.op("dve", lambda e: e.tensor_scalar(out=t1[:], in0=arate[:], scalar1=-1.0, scalar2=prm[:, 9:10], op0=ALU.add, op1=ALU.mult),
              reads=[arate.b, prm.b], writes=[t1.b])
        cx.op("dve", lambda e: e.scalar_tensor_tensor(out=kmod[:], in0=t1[:], scalar=1.0, in1=sh["k"][:], op0=ALU.add, op1=ALU.mult),
              reads=[t1.b, sh["k"].b], writes=[kmod.b])
        cx.op("dve", lambda e: e.scalar_tensor_tensor(out=t1[:], in0=sh["r"][:], scalar=prm[:, 10:11], in1=kmod[:], op0=ALU.mult, op1=ALU.mult),
              reads=[sh["r"].b, prm.b, kmod.b], writes=[t1.b])
        p = rr("pp", pp)
        cx.op("pe", lambda e: e.matmul(p[:], lhsT=bones[:], rhs=t1[:], start=True, stop=True), reads=[bones.b, t1.b], writes=[p.b])
        cx.op("dve", lambda e: e.tensor_tensor(out=bonus[:], in0=p[:], in1=v[:], op=ALU.mult), reads=[p.b, v.b], writes=[bonus.b])
        cx.op("dve", lambda e: e.tensor_tensor_scan(out=cs[:], data0=scanm[:], data1=sig[:], initial=0.0, op0=ALU.mult, op1=ALU.add),
              reads=[scanm.b, sig.b], writes=[cs.b])
        cx.op("act", lambda e: e.activation(out=epos[:], in_=cs[:], func=AF.Exp, scale=-C0), reads=[cs.b], writes=[epos.b])
        cx.op("act", lambda e: e.activation(out=eneg[:], in_=cs[:], func=AF.Exp, scale=C0), reads=[cs.b], writes=[eneg.b])
        cx.op("dve", lambda e: e.tensor_tensor(out=t1[:], in0=cs[:], in1=sig[:], op=ALU.subtract), reads=[cs.b, sig.b], writes=[t1.b])
        cx.op("act", lambda e: e.activation(out=eprev[:], in_=t1[:], func=AF.Exp, scale=-C0), reads=[t1.b], writes=[eprev.b])
        cx.op("dve", lambda e: e.tensor_copy(out=ec[:], in_=c3(epos)[:, :, 63]), reads=[epos.b], writes=[ec.b])
        cx.op("dve", lambda e: e.tensor_tensor(out=t2[:], in0=kk[:], in1=arate[:], op=ALU.mult), reads=[kk.b, arate.b], writes=[t2.b])
        cx.op("dve", lambda e: e.tensor_tensor(out=bneg[:], in0=t2[:], in1=eneg[:], op=ALU.mult), reads=[t2.b, eneg.b], writes=[bneg.b])
        cx.op("dve", lambda e: e.tensor_tensor(out=kneg[:], in0=kmod[:], in1=eneg[:], op=ALU.mult), reads=[kmod.b, eneg.b], writes=[kneg.b])
        for (psl, csl) in HALF:
            eng = "dve"
            cx.op(eng, lambda e: e.scalar_tensor_tensor(out=bd["a"][psl, :, csl], in0=c3(kk)[psl], scalar=-1.0, in1=c3(eprev)[psl],
                                                        op0=ALU.mult, op1=ALU.mult), reads=[kk.b, eprev.b], writes=[bd["a"].b])
            cx.op(eng, lambda e: e.tensor_copy(out=bd["b"][psl, :, csl], in_=c3(bneg)[psl]), reads=[bneg.b], writes=[bd["b"].b])
            cx.op(eng, lambda e: e.tensor_copy(out=bd["k"][psl, :, csl], in_=c3(kneg)[psl]), reads=[kneg.b], writes=[bd["k"].b])
            cx.op(eng, lambda e: e.tensor_tensor(out=bd["r"][psl, :, csl], in0=c3(sh["r"])[psl], in1=c3(epos)[psl], op=ALU.mult),
                  reads=[sh["r"].b, epos.b], writes=[bd["r"].b])
            cx.op(eng, lambda e: e.tensor_copy(out=bd["v"][psl, :, csl], in_=c3(v)[psl]), reads=[v.b], writes=[bd["v"].b])
            ecb = ec[psl, :].unsqueeze(2).broadcast_to([64, 8, 64])
            cx.op(eng, lambda e: e.tensor_tensor(out=bd["bh"][psl, :, csl], in0=c3(bneg)[psl], in1=ecb, op=ALU.mult),
                  reads=[bneg.b, ec.b], writes=[bd["bh"].b])
            cx.op(eng, lambda e: e.tensor_tensor(out=bd["kh"][psl, :, csl], in0=c3(kneg)[psl], in1=ecb, op=ALU.mult),
                  reads=[kneg.b, ec.b], writes=[bd["kh"].b])
        def chunk_gen(c):
            ct = cs_[c % 2]
            aT, bT, kT, rT, vT = (bd[q][:, c, :] for q in ["a", "b", "k", "r", "v"])
            bdr = [bd[q].b for q in ["a", "b", "k", "r", "v", "bh", "kh"]]

            def mm_mask(lhsT, rhs, mask, out_t):
                p = rr("pp", pp)
                cx.op("pe", lambda e: e.matmul(p[:, 0:128], lhsT=lhsT, rhs=rhs, start=True, stop=True), reads=bdr, writes=[p.b])
                cx.op("dve", lambda e: e.tensor_tensor(out=out_t[:], in0=p[:, 0:128], in1=mask[:], op=ALU.mult),
                      reads=[p.b, mask.b], writes=[out_t.b])

            mm_mask(bT, aT, msl, ct["LT"])
            yield
            mm_mask(aT, bT, mslT, ct["L"])
            yield
            mm_mask(kT, aT, msl, ct["AkT"])
            yield
            mm_mask(bT, rT, mil, ct["ArbT"])
            yield
            mm_mask(kT, rT, mil, ct["ArkT"])
            yield
            for i, q in enumerate(["a", "v", "bh", "kh"]):
                cx.op("pe", lambda e: e.transpose(out=ptb[:, i * 128:(i + 1) * 128], in_=bd[q][:, c, :], identity=identb[:]),
                      reads=[bd[q].b, identb.b], writes=[ptb.b], inc=(i == 3))
            tok = ct["tok"]
            cx.op("act", lambda e: e.activation(out=tok[:].rearrange("p a b -> p (a b)"), in_=ptb[:, 0:512], func=AF.Copy),
                  reads=[ptb.b], writes=[tok.b])
            Atok, Vtok, Bh, Kh = (tok[:, i, :] for i in range(4))
            yield
            X, Xb = ct["X"], ct["Xb"]
            p = rr("pp", pp)
            cx.op("pe", lambda e: e.matmul(p[:, 0:128], lhsT=ct["AkT"][:], rhs=Vtok, start=True, stop=True),
                  reads=[ct["AkT"].b, tok.b], writes=[p.b])
            cx.op("act", lambda e: e.activation(out=X[:, 128:256], in_=p[:, 0:128], func=AF.Copy), reads=[p.b], writes=[X.b])
            cx.op("dve", lambda e: e.tensor_copy(out=X[:, 0:128], in_=Atok), reads=[tok.b], writes=[X.b])
            cx.op("dve", lambda e: e.tensor_copy(out=Xb[:], in_=X[:]), reads=[X.b], writes=[Xb.b])
            Pk, PTk = ct["L"], ct["LT"]
            yield
            for lev in range(6):
                p = rr("pp", pp)
                cx.op("pe", lambda e: e.matmul(p[:, 0:256], lhsT=PTk[:], rhs=Xb[:], start=True, stop=True),
                      reads=[PTk.b, Xb.b], writes=[p.b])
                cx.op("dve", lambda e: e.tensor_tensor(out=X[:], in0=X[:], in1=p[:, 0:256], op=ALU.add), reads=[X.b, p.b], writes=[X.b])
                cx.op("act", lambda e: e.activation(out=Xb[:], in_=X[:], func=AF.Copy), reads=[X.b], writes=[Xb.b])
                if lev < 5:
                    Pn, PTn = (ct["P1"], ct["PT1"]) if lev % 2 == 0 else (ct["P2"], ct["PT2"])
                    p1 = rr("pp", pp)
                    cx.op("pe", lambda e: e.matmul(p1[:, 0:128], lhsT=PTk[:], rhs=Pk[:], start=True, stop=True),
                          reads=[PTk.b, Pk.b], writes=[p1.b])
                    cx.op("act", lambda e: e.activation(out=Pn[:], in_=p1[:, 0:128], func=AF.Copy), reads=[p1.b], writes=[Pn.b])
                    p2 = rr("pp", pp)
                    cx.op("pe", lambda e: e.matmul(p2[:, 0:128], lhsT=Pk[:], rhs=PTk[:], start=True, stop=True),
                          reads=[PTk.b, Pk.b], writes=[p2.b])
                    cx.op("dve", lambda e: e.tensor_copy(out=PTn[:], in_=p2[:, 0:128]), reads=[p2.b], writes=[PTn.b])
                    Pk, PTk = Pn, PTn
                yield
            p = rr("pp", pp)
            cx.op("pe", lambda e: e.matmul(p[:, 0:128], lhsT=Xb[:, 0:128], rhs=Bh, start=True, stop=True), reads=[Xb.b, tok.b], writes=[p.b])
            cx.op("dve", lambda e: e.scalar_tensor_tensor(out=ct["MT"][:], in0=identf[:], scalar=ec[:, c:c + 1], in1=p[:, 0:128],
                                                          op0=ALU.mult, op1=ALU.add), reads=[identf.b, ec.b, p.b], writes=[ct["MT"].b])
            yield
            p = rr("pp", pp)
            cx.op("pe", lambda e: e.matmul(p[:, 0:128], lhsT=Bh, rhs=Xb[:, 128:256], start=True, stop=False), reads=[Xb.b, tok.b], writes=[p.b], inc=False)
            cx.op("pe", lambda e: e.matmul(p[:, 0:128], lhsT=Kh, rhs=Vtok, start=False, stop=True), reads=[tok.b], writes=[p.b])
            cx.op("act", lambda e: e.activation(out=ct["Nc"][:], in_=p[:, 0:128], func=AF.Copy), reads=[p.b], writes=[ct["Nc"].b])
            p = rr("pp", pp)
            cx.op("pe", lambda e: e.matmul(p[:, 0:128], lhsT=Xb[:, 0:128], rhs=ct["ArbT"][:], start=True, stop=True),
                  reads=[Xb.b, ct["ArbT"].b], writes=[p.b])
            cx.op("dve", lambda e: e.tensor_tensor(out=ct["RbT"][:], in0=p[:, 0:128], in1=rT, op=ALU.add), reads=[p.b, bd["r"].b], writes=[ct["RbT"].b])
            yield
            p = rr("pp", pp)
            cx.op("pe", lambda e: e.matmul(p[:, 0:128], lhsT=Hb[:], rhs=ct["RbT"][:], start=True, stop=False), reads=[Hb.b, ct["RbT"].b], writes=[p.b], inc=False)
            cx.op("pe", lambda e: e.matmul(p[:, 0:128], lhsT=Xb[:, 128:256], rhs=ct["ArbT"][:], start=False, stop=False),
                  reads=[Xb.b, ct["ArbT"].b], writes=[p.b], inc=False)
            cx.op("pe", lambda e: e.matmul(p[:, 0:128], lhsT=Vtok, rhs=ct["ArkT"][:], start=False, stop=True), reads=[tok.b, ct["ArkT"].b], writes=[p.b])
            for (psl, csl) in HALF:
                cx.op("act", lambda e: e.activation(out=ysc[psl, c * 64:(c + 1) * 64], in_=p[psl, csl], func=AF.Copy), reads=[p.b], writes=[ysc.b])
            yield
            p = rr("pp", pp)
            cx.op("pe", lambda e: e.matmul(p[:, 0:128], lhsT=ct["MT"][:], rhs=Hb[:], start=True, stop=True), reads=[ct["MT"].b, Hb.b], writes=[p.b])
            cx.op("dve", lambda e: e.tensor_tensor(out=Hb[:], in0=p[:, 0:128], in1=ct["Nc"][:], op=ALU.add), reads=[p.b, ct["Nc"].b], writes=[Hb.b])
        for c0 in range(0, 8, 2):
            gens = [chunk_gen(c0), chunk_gen(c0 + 1)]
            for _ in range(3):
                next(gens[0])
            while gens:
                for g_ in list(gens):
                    try:
                        next(g_)
                    except StopIteration:
                        gens.remove(g_)
        p = rr("pp", pp)
        cx.op("pe", lambda e: e.matmul(p[:], lhsT=bones[:], rhs=ysc[:], start=True, stop=True), reads=[bones.b, ysc.b], writes=[p.b])
        cx.op("dve", lambda e: e.scalar_tensor_tensor(out=yc[:], in0=p[:], scalar=-1.0 / 64, in1=ysc[:], op0=ALU.mult, op1=ALU.add),
              reads=[p.b, ysc.b], writes=[yc.b])
        cx.op("act", lambda e: e.activation(out=t1[:], in_=yc[:], func=AF.Square), reads=[yc.b], writes=[t1.b])
        p = rr("pp", pp)
        cx.op("pe", lambda e: e.matmul(p[:], lhsT=bones[:], rhs=t1[:], start=True, stop=True), reads=[bones.b, t1.b], writes=[p.b])
        cx.op("act", lambda e: e.activation(out=t1[:], in_=p[:], func=AF.Sqrt, bias=cst[:, 0:1], scale=1.0 / 64), reads=[p.b, cst.b], writes=[t1.b])
        cx.op("dve", lambda e: e.reciprocal(out=t1[:], in_=t1[:]), reads=[t1.b], writes=[t1.b])
        cx.op("dve", lambda e: e.tensor_tensor(out=yc[:], in0=yc[:], in1=t1[:], op=ALU.mult), reads=[yc.b, t1.b], writes=[yc.b])
        cx.op("dve", lambda e: e.tensor_scalar(out=yc[:], in0=yc[:], scalar1=prm[:, 11:12], scalar2=prm[:, 12:13], op0=ALU.mult, op1=ALU.add),
              reads=[yc.b, prm.b], writes=[yc.b])
        cx.op("dve", lambda e: e.tensor_tensor(out=yc[:], in0=yc[:], in1=bonus[:], op=ALU.add), reads=[yc.b, bonus.b], writes=[yc.b])
        mo_t = mo[s % 2]
        cx.op("dve", lambda e: e.tensor_tensor(out=mo_t[:], in0=yc[:], in1=gout[:], op=ALU.mult), reads=[yc.b, gout.b], writes=[mo_t.b])
        cx.dma("sp", mix_out[128:256, tsl], mo_t[:], reads=[mo_t.b], chan=f"Rmo{s % 2}")


_PROG = {}


def _prog(key, fn):
    if key not in _PROG:
        _PROG[key] = fn()
    return _PROG[key]


def _wr(w, a, b):
    return np.ascontiguousarray(w.reshape(a, 128, b, 128).transpose(2, 1, 0, 3).reshape(b, 128, a * 128))


def _gcol(v):
    return np.ascontiguousarray(v.reshape(NDC, 128).T)


def kernel(**inputs):
    inp = {k: np.asarray(v) for k, v in inputs.items()}
    cores = list(range(NCORES))
    x = inp["x"][0]
    xT = [np.ascontiguousarray(x[c * TC:(c + 1) * TC].T).reshape(NDC, 128, TC) for c in cores]
    nc = _prog("Epre", lambda: build_E(False, True))
    g0 = _gcol(inp["norm_mix"][0])
    res = run_bass_kernel_spmd(nc, [{"xT": xT[c], "g_nxt": g0} for c in cores], core_ids=cores).results
    hs = [np.asarray(res[c]["ho"]) for c in cores]
    perm = np.concatenate([np.concatenate([np.arange(c * 128, c * 128 + 128), 1024 + np.arange(c * 128, c * 128 + 128)])
                           for c in cores])
    vfirst = None
    for l in range(DEPTH):
        hT = np.ascontiguousarray(np.concatenate(hs, axis=2))
        ncM = _prog("M", lambda: build_M(1))
        ins = []
        for c in cores:
            dd = attn_inputs(inp, l, c)
            dd.update(rwkv_inputs(inp, l, c))
            dd["hT"] = hT
            dd["vfirst"] = vfirst[c] if l > 0 else np.zeros((128, T), np.float32)
            ins.append(dd)
        res = run_bass_kernel_spmd(ncM, ins, core_ids=cores).results
        if l == 0:
            vfirst = [np.asarray(res[c]["vfirst_o"]) for c in cores]
        mixT = np.concatenate([np.asarray(res[c]["mix_out"]) for c in cores], axis=0)
        del ins
        last = (l == DEPTH - 1)
        ncE = _prog("E", lambda: build_E(True, True))
        w_out = _wr(inp["w_out"][l][perm], NDC, NDC)
        w_up = _wr(inp["mlp_up"][l], NDC, NFC)
        w_dn = _wr(inp["mlp_down"][l], NFC, NDC)
        gm = _gcol(inp["norm_mlp"][l])
        ins = []
        for c in cores:
            dd = {"xT": xT[c], "mixT": np.ascontiguousarray(mixT[:, c * TC:(c + 1) * TC]).reshape(NDC, 128, TC),
                  "w_out": w_out, "w_up": w_up, "w_dn": w_dn, "g_mlp": gm}
            dd["g_nxt"] = _gcol(inp["norm_mix"][l + 1]) if not last else np.ones((128, NDC), np.float32)
            ins.append(dd)
        res = run_bass_kernel_spmd(ncE, ins, core_ids=cores).results
        xT = [np.asarray(res[c]["xo"]) for c in cores]
        if not last:
            hs = [np.asarray(res[c]["ho"]) for c in cores]
        del ins, w_out, w_up, w_dn
    out = np.concatenate([xT[c].reshape(D, TC).T for c in cores], axis=0)[None]
    return np.ascontiguousarray(out).astype(np.float32)
```
